# Optimizing a Trainium2 kernel written in Bass

```python
import jax, jax.numpy as jnp
from jax import lax
import numpy as np

D_MODEL = 1024
BATCH = 8
SEQ = 4096
DEPTH = 1

MEM_LEN = 256
EPS = 1e-6
MIX_WIDTH = D_MODEL
RNN_WIDTH = MIX_WIDTH // 2
RNN_HEADS = 8
RNN_HEAD_DIM = RNN_WIDTH // RNN_HEADS
CONV_WIDTH = 4
LRU_C = 8.0
SB_WIDTH = MIX_WIDTH - RNN_WIDTH
SB_HEADS = 8
SB_HEAD_DIM = SB_WIDTH // SB_HEADS
Q_BLOCK = 128
PROJ_WIDTH = 2 * RNN_WIDTH + 3 * SB_WIDTH
XA_HEADS = 4
XA_HEAD_DIM = D_MODEL // XA_HEADS
N_GROUPS = 4
EXPERTS_PER_GROUP = 4
TOP_K = 2
D_EXPERT = D_MODEL // 4

kernel_name = "hymba_rglru_stickbreak_hmoe_block"


def rms_norm(x, gain):
    xf = x.astype(jnp.float32)
    var = jnp.mean(xf * xf, axis=-1, keepdims=True)
    return (xf * lax.rsqrt(var + EPS) * gain.astype(jnp.float32)).astype(x.dtype)


def causal_depthwise_conv(x, w, b):
    c = x.shape[-1]
    y = lax.conv_general_dilated(
        x, w[:, None, :].astype(x.dtype), window_strides=(1,),
        padding=[(CONV_WIDTH - 1, 0)], dimension_numbers=('NWC', 'WIO', 'NWC'),
        feature_group_count=c)
    return y + b.astype(x.dtype)


def rg_lru(x, w_a, b_a, w_x, b_x, lam):
    bsz, s, _ = x.shape
    xh = x.reshape(bsz, s, RNN_HEADS, RNN_HEAD_DIM)
    r = jax.nn.sigmoid(jnp.einsum('bshi,hij->bshj', xh, w_a) + b_a).reshape(bsz, s, RNN_WIDTH)
    i = jax.nn.sigmoid(jnp.einsum('bshi,hij->bshj', xh, w_x) + b_x).reshape(bsz, s, RNN_WIDTH)
    log_a = -LRU_C * r.astype(jnp.float32) * jax.nn.softplus(-lam.astype(jnp.float32))
    a = jnp.exp(log_a)
    b = jnp.sqrt(-jnp.expm1(2.0 * log_a)) * (i * x).astype(jnp.float32)

    def combine(left, right):
        a_l, b_l = left
        a_r, b_r = right
        return a_l * a_r, a_r * b_l + b_r

    _, h = lax.associative_scan(combine, (a, b), axis=1)
    return h.astype(x.dtype)


def stick_breaking_attention(q, k, v):
    bsz, s, nh, dh = q.shape
    n_blocks = s // Q_BLOCK
    scale = dh ** -0.5
    q_blocks = q.reshape(bsz, n_blocks, Q_BLOCK, nh, dh).transpose(1, 0, 3, 2, 4)
    key_pos = jnp.arange(s)

    def one_block(args):
        q_blk, blk = args
        query_pos = blk * Q_BLOCK + jnp.arange(Q_BLOCK)
        z = jnp.einsum('bhqd,bkhd->bhqk', q_blk, k).astype(jnp.float32) * scale
        mask = key_pos[None, :] < query_pos[:, None]
        log_not = jnp.where(mask, jax.nn.log_sigmoid(-z), 0.0)
        later = lax.cumsum(log_not, axis=3, reverse=True) - log_not
        w = jnp.where(mask, jnp.exp(jax.nn.log_sigmoid(z) + later), 0.0)
        return jnp.einsum('bhqk,bkhd->bqhd', w.astype(v.dtype), v)

    out = lax.map(one_block, (q_blocks, jnp.arange(n_blocks)))
    return out.transpose(1, 0, 2, 3, 4).reshape(bsz, s, nh * dh)


def hybrid_mixer(h, w_in, conv_w, conv_b, w_a, b_a, w_x, b_x, lam, g_rnn, g_sb, w_out):
    bsz, s, _ = h.shape
    proj = h @ w_in
    x_rnn, gate_rnn, q, k, v = jnp.split(
        proj, [RNN_WIDTH, 2 * RNN_WIDTH, 2 * RNN_WIDTH + SB_WIDTH, 2 * RNN_WIDTH + 2 * SB_WIDTH], axis=-1)
    x_rnn = causal_depthwise_conv(x_rnn, conv_w, conv_b)
    y_rnn = rg_lru(x_rnn, w_a, b_a, w_x, b_x, lam) * jax.nn.gelu(gate_rnn)
    hd = (bsz, s, SB_HEADS, SB_HEAD_DIM)
    y_sb = stick_breaking_attention(q.reshape(hd), k.reshape(hd), v.reshape(hd))
    y = jnp.concatenate([rms_norm(y_rnn, g_rnn), rms_norm(y_sb, g_sb)], axis=-1)
    return y @ w_out


def memory_cross_attention(h, m, w_q, w_k, w_v, w_o):
    bsz, s, d = h.shape
    n_mem = m.shape[1]
    q = (h @ w_q).reshape(bsz, s, XA_HEADS, XA_HEAD_DIM)
    k = (m @ w_k).reshape(bsz, n_mem, XA_HEADS, XA_HEAD_DIM)
    v = (m @ w_v).reshape(bsz, n_mem, XA_HEADS, XA_HEAD_DIM)
    scores = jnp.einsum('bshd,bmhd->bhsm', q, k).astype(jnp.float32) * (XA_HEAD_DIM ** -0.5)
    p = jax.nn.softmax(scores, axis=-1).astype(v.dtype)
    o = jnp.einsum('bhsm,bmhd->bshd', p, v).reshape(bsz, s, d)
    return o @ w_o


def hierarchical_moe(h, w_group_router, b_group_router, w_expert_router, b_expert_router,
                     w_gate, w_up, w_down):
    bsz, s, d = h.shape
    t = h.reshape(-1, d)
    group_prob = jax.nn.softmax((t @ w_group_router).astype(jnp.float32) + b_group_router, axis=-1)
    group_p, group_idx = lax.top_k(group_prob, 1)
    expert_logits = jnp.einsum('nd,gde->nge', t, w_expert_router).astype(jnp.float32) + b_expert_router
    chosen = jnp.take_along_axis(expert_logits, group_idx[:, :, None], axis=1)[:, 0]
    top_logit, top_idx = lax.top_k(chosen, TOP_K)
    top_w = jax.nn.softmax(top_logit, axis=-1) * group_p
    expert_w = jnp.sum(jax.nn.one_hot(top_idx, EXPERTS_PER_GROUP, dtype=jnp.float32) * top_w[..., None], axis=1)
    combine = jax.nn.one_hot(group_idx[:, 0], N_GROUPS, dtype=jnp.float32)[:, :, None] * expert_w[:, None, :]
    out = jnp.zeros_like(t)
    for g in range(N_GROUPS):
        hid = jax.nn.silu(jnp.einsum('nd,edf->nef', t, w_gate[g])) * jnp.einsum('nd,edf->nef', t, w_up[g])
        hid = hid * combine[:, g, :, None].astype(hid.dtype)
        out = out + jnp.einsum('nef,efd->nd', hid, w_down[g])
    return out.reshape(bsz, s, d)


def setup_inputs(seed: int = 0) -> dict:
    key = jax.random.key(seed)
    ks = jax.random.split(key, 32)
    f32 = jnp.float32
    L = DEPTH

    def nrm(k, shape, fan_in):
        return jax.random.normal(k, shape, f32) * (fan_in ** -0.5)

    def gain(k, shape):
        return 1.0 + 0.02 * jax.random.normal(k, shape, f32)

    u = jax.random.uniform(ks[9], (L, RNN_WIDTH), f32, 0.9, 0.999)
    a0 = u ** (1.0 / LRU_C)
    lam = jnp.log(a0) - jnp.log1p(-a0)
    return {
        "x": jax.random.normal(ks[0], (BATCH, SEQ, D_MODEL), f32),
        "mem": jax.random.normal(ks[1], (BATCH, MEM_LEN, D_MODEL), f32),
        "norm_mix": gain(ks[2], (L, D_MODEL)),
        "w_in": nrm(ks[3], (L, D_MODEL, PROJ_WIDTH), D_MODEL),
        "conv_w": nrm(ks[4], (L, CONV_WIDTH, RNN_WIDTH), CONV_WIDTH),
        "conv_b": 0.01 * jax.random.normal(ks[5], (L, RNN_WIDTH), f32),
        "lru_w_a": nrm(ks[6], (L, RNN_HEADS, RNN_HEAD_DIM, RNN_HEAD_DIM), RNN_HEAD_DIM),
        "lru_b_a": 0.01 * jax.random.normal(ks[7], (L, RNN_HEADS, RNN_HEAD_DIM), f32),
        "lru_w_x": nrm(ks[8], (L, RNN_HEADS, RNN_HEAD_DIM, RNN_HEAD_DIM), RNN_HEAD_DIM),
        "lru_b_x": 0.01 * jax.random.normal(ks[10], (L, RNN_HEADS, RNN_HEAD_DIM), f32),
        "lru_lambda": lam,
        "norm_rnn_out": gain(ks[11], (L, RNN_WIDTH)),
        "norm_sb_out": gain(ks[12], (L, SB_WIDTH)),
        "w_out": nrm(ks[13], (L, MIX_WIDTH, D_MODEL), MIX_WIDTH),
        "norm_xattn": gain(ks[14], (L, D_MODEL)),
        "norm_mem": gain(ks[15], (L, D_MODEL)),
        "xa_w_q": nrm(ks[16], (L, D_MODEL, D_MODEL), D_MODEL),
        "xa_w_k": nrm(ks[17], (L, D_MODEL, D_MODEL), D_MODEL),
        "xa_w_v": nrm(ks[18], (L, D_MODEL, D_MODEL), D_MODEL),
        "xa_w_o": nrm(ks[19], (L, D_MODEL, D_MODEL), D_MODEL),
        "norm_moe": gain(ks[20], (L, D_MODEL)),
        "w_group_router": nrm(ks[21], (L, D_MODEL, N_GROUPS), D_MODEL),
        "b_group_router": 0.01 * jax.random.normal(ks[22], (L, N_GROUPS), f32),
        "w_expert_router": nrm(ks[23], (L, N_GROUPS, D_MODEL, EXPERTS_PER_GROUP), D_MODEL),
        "b_expert_router": 0.01 * jax.random.normal(ks[24], (L, N_GROUPS, EXPERTS_PER_GROUP), f32),
        "w_gate": nrm(ks[25], (L, N_GROUPS, EXPERTS_PER_GROUP, D_MODEL, D_EXPERT), D_MODEL),
        "w_up": nrm(ks[26], (L, N_GROUPS, EXPERTS_PER_GROUP, D_MODEL, D_EXPERT), D_MODEL),
        "w_down": nrm(ks[27], (L, N_GROUPS, EXPERTS_PER_GROUP, D_EXPERT, D_MODEL), D_EXPERT),
        "norm_final": gain(ks[28], (D_MODEL,)),
    }


def reference(x, mem, norm_mix, w_in, conv_w, conv_b, lru_w_a, lru_b_a, lru_w_x, lru_b_x,
              lru_lambda, norm_rnn_out, norm_sb_out, w_out, norm_xattn, norm_mem,
              xa_w_q, xa_w_k, xa_w_v, xa_w_o, norm_moe, w_group_router, b_group_router,
              w_expert_router, b_expert_router, w_gate, w_up, w_down, norm_final):
    for l in range(DEPTH):
        x = x + hybrid_mixer(rms_norm(x, norm_mix[l]), w_in[l], conv_w[l], conv_b[l],
                             lru_w_a[l], lru_b_a[l], lru_w_x[l], lru_b_x[l], lru_lambda[l],
                             norm_rnn_out[l], norm_sb_out[l], w_out[l])
        x = x + memory_cross_attention(rms_norm(x, norm_xattn[l]), rms_norm(mem, norm_mem[l]),
                                       xa_w_q[l], xa_w_k[l], xa_w_v[l], xa_w_o[l])
        x = x + hierarchical_moe(rms_norm(x, norm_moe[l]), w_group_router[l], b_group_router[l],
                                 w_expert_router[l], b_expert_router[l],
                                 w_gate[l], w_up[l], w_down[l])
    return rms_norm(x, norm_final)
```

```python
import numpy as np
import concourse.bass as bass
import concourse.mybir as mybir
from concourse.bass_utils import run_bass_kernel_spmd
from contextlib import ExitStack

F32 = mybir.dt.float32
BF16 = mybir.dt.bfloat16
AF = mybir.ActivationFunctionType
ALU = mybir.AluOpType
AX = mybir.AxisListType

S_LEN = 4096
D = 1024
NT = 8
EPS = 1e-6
NEG = -30000.0


class Buf:
    __slots__ = ("name", "w", "r", "dsem")

    def __init__(self, name):
        self.name = name
        self.w = None
        self.r = {}
        self.dsem = None


class Rot:
    def __init__(self, tiles, name):
        self.t = tiles
        self.b = [Buf("%s%d" % (name, i)) for i in range(len(tiles))]
        self.i = 0

    def next(self):
        k = self.i % len(self.t)
        self.i += 1
        return self.t[k], self.b[k]


class Sched:
    ENGS = ("pe", "act", "dve", "pool", "sp")
    EMAP = {"pe": "tensor", "act": "scalar", "dve": "vector", "pool": "gpsimd", "sp": "sync"}

    def __init__(self, nc, es):
        self.nc = nc
        self.es = es
        self.prog = {e: [] for e in self.ENGS}
        self.sems = {}
        self.cnt = {}
        self.seen = {e: {} for e in self.ENGS}
        for e in self.ENGS:
            self._mksem("E_" + e)
        self.ndma = 0
        self.ninstr = 0

    def _mksem(self, key):
        self.sems[key] = self.es.enter_context(self.nc.semaphore("s_" + key))
        self.cnt[key] = 0
        return key

    def _need(self, reads, writes):
        need = {}
        for b in reads:
            if b.w is not None and need.get(b.w[0], 0) < b.w[1]:
                need[b.w[0]] = b.w[1]
        for b in writes:
            if b.w is not None and need.get(b.w[0], 0) < b.w[1]:
                need[b.w[0]] = b.w[1]
            for k, v in b.r.items():
                if need.get(k, 0) < v:
                    need[k] = v
        return need

    def _waits(self, eng, need):
        waits = []
        seen = self.seen[eng]
        for k, v in need.items():
            if seen.get(k, 0) < v:
                seen[k] = v
                waits.append((k, v))
        return waits

    def _force_inc(self, k, v, eng):
        e = k[2:]
        prog = self.prog[e]
        if v != self.cnt[k] + 1 or not prog or prog[-1][2] is not None or prog[-1][1] is None:
            raise RuntimeError("forward dep on %s from %s" % (k, eng))
        prog[-1][2] = (k, 1)
        self.cnt[k] += 1

    def op(self, eng, fn, reads=(), writes=(), inc=True):
        key = "E_" + eng
        need = self._need(reads, writes)
        if key in need and need[key] > self.cnt[key]:
            if eng != "pe":
                raise RuntimeError("same-engine forward dep on " + eng)
            del need[key]
        for k, v in need.items():
            if k.startswith("E_") and v > self.cnt[k]:
                self._force_inc(k, v, eng)
        waits = self._waits(eng, need)
        stamp = (key, self.cnt[key] + 1)
        if inc:
            self.cnt[key] += 1
        self.prog[eng].append([waits, fn, (key, 1) if inc else None])
        self.ninstr += 1
        for b in reads:
            if b.r.get(stamp[0], 0) < stamp[1]:
                b.r[stamp[0]] = stamp[1]
        for b in writes:
            b.w = stamp
            b.r = {}

    def dma(self, eng, out_ap, in_ap, reads=(), writes=(), serialize=True):
        sb = writes[0]
        if sb.dsem is None:
            sb.dsem = self._mksem("D%d_%s" % (self.ndma, sb.name))
            self.ndma += 1
        k = sb.dsem
        need = self._need(reads, writes)
        if not serialize and k in need:
            del need[k]
        for kk, v in need.items():
            if kk.startswith("E_") and v > self.cnt[kk]:
                self._force_inc(kk, v, eng)
        waits = self._waits(eng, need)
        self.cnt[k] += 16
        stamp = (k, self.cnt[k])

        def fn(e, out_ap=out_ap, in_ap=in_ap):
            return e.dma_start(out=out_ap, in_=in_ap)
        self.prog[eng].append([waits, fn, (k, 16)])
        self.ninstr += 1
        for b in reads:
            if b.r.get(k, 0) < stamp[1]:
                b.r[k] = stamp[1]
        for b in writes:
            b.w = stamp
            b.r = {}

    def barrier(self):
        for e in self.ENGS:
            waits = []
            for k, v in self.cnt.items():
                if v > 0 and k != "E_" + e and self.seen[e].get(k, 0) < v:
                    self.seen[e][k] = v
                    waits.append((k, v))
            own = "E_" + e
            if self.cnt[own] > 0 and self.seen[e].get(own, 0) < self.cnt[own]:
                self.seen[e][own] = self.cnt[own]
                waits.append((own, self.cnt[own]))
            self.prog[e].append([waits, None, None])

    def emit(self):
        with self.nc.Block() as block:
            for e in self.ENGS:
                prog = self.prog[e]
                if not prog:
                    continue

                def body(eng, prog=prog):
                    for waits, fn, inc in prog:
                        for k, v in waits:
                            eng.wait_ge(self.sems[k], v)
                        if fn is None:
                            continue
                        ins = fn(eng)
                        if inc is not None:
                            ins.then_inc(self.sems[inc[0]], inc[1])
                getattr(block, self.EMAP[e])(body)
        self.prog = {e: [] for e in self.ENGS}


class K:
    def __init__(self, S):
        self.S = S

    def act(self, out, in_, func, reads, writes, **kw):
        self.S.op("act", lambda e: e.activation(out=out, in_=in_, func=func, **kw), reads, writes)

    def mm(self, out, lhsT, rhs, start, stop, reads, writes, inc, skip=False):
        self.S.op("pe", lambda e: e.matmul(out, lhsT=lhsT, rhs=rhs, start=start, stop=stop,
                                           skip_group_check=skip), reads, writes, inc=inc)

    def tr(self, out, in_, ident, reads, writes, inc):
        self.S.op("pe", lambda e: e.transpose(out=out, in_=in_, identity=ident), reads, writes, inc=inc)

    def copy(self, eng, out, in_, reads, writes):
        if eng == "act":
            self.S.op("act", lambda e: e.activation(out=out, in_=in_, func=AF.Copy), reads, writes)
        else:
            self.S.op(eng, lambda e: e.tensor_copy(out=out, in_=in_), reads, writes)

    def tt(self, eng, out, in0, in1, op, reads, writes):
        self.S.op(eng, lambda e: e.tensor_tensor(out=out, in0=in0, in1=in1, op=op), reads, writes)

    def ts(self, eng, out, in0, s1, s2, op0, op1, reads, writes):
        if op1 is None:
            self.S.op(eng, lambda e: e.tensor_scalar(out=out, in0=in0, scalar1=s1, scalar2=None, op0=op0),
                      reads, writes)
        else:
            self.S.op(eng, lambda e: e.tensor_scalar(out=out, in0=in0, scalar1=s1, scalar2=s2, op0=op0, op1=op1),
                      reads, writes)

    def stt(self, out, in0, scalar, in1, op0, op1, reads, writes, accum_out=None):
        if accum_out is None:
            self.S.op("dve", lambda e: e.scalar_tensor_tensor(out=out, in0=in0, scalar=scalar, in1=in1,
                                                              op0=op0, op1=op1), reads, writes)
        else:
            self.S.op("dve", lambda e: e.scalar_tensor_tensor(out=out, in0=in0, scalar=scalar, in1=in1,
                                                              op0=op0, op1=op1, accum_out=accum_out), reads, writes)

    def memset(self, eng, ap, val, writes):
        self.S.op(eng, lambda e: e.memset(ap, val), (), writes)

    def asel(self, out, in_, pattern, op, fill, base, cm, reads, writes):
        self.S.op("pool", lambda e: e.affine_select(out=out, in_=in_, pattern=pattern, compare_op=op,
                                                    fill=fill, base=base, channel_multiplier=cm), reads, writes)

    def rmax(self, out, in_, reads, writes):
        self.S.op("dve", lambda e: e.reduce_max(out=out, in_=in_, axis=AX.X), reads, writes)

    def recip(self, out, in_, reads, writes):
        self.S.op("dve", lambda e: e.reciprocal(out=out, in_=in_), reads, writes)

    def scan(self, out, d0, d1, init, reads, writes):
        self.S.op("dve", lambda e: e.tensor_tensor_scan(out=out, data0=d0, data1=d1, initial=init,
                                                        op0=ALU.mult, op1=ALU.add), reads, writes)


def run_merged(main, side):
    n, m = len(main), len(side)
    done = 0
    for i, f in enumerate(main):
        f()
        want = (m * (i + 1)) // max(n, 1)
        while done < want:
            side[done]()
            done += 1
    while done < m:
        side[done]()
        done += 1


class Defer:
    def __init__(self, k, S, ops):
        self._k, self._S, self._ops = k, S, ops

    def dma(self, *a, **kw):
        self._ops.append(lambda: self._S.dma(*a, **kw))

    def __getattr__(self, name):
        f = getattr(self._k, name)

        def wrap(*a, **kw):
            self._ops.append(lambda: f(*a, **kw))
        return wrap


def build(stop_after=3, dbg=False):
    nc = bass.Bass("TRN2", target_bir_lowering=False)

    def din(name, shape, dt=F32):
        return nc.dram_tensor(name, shape, dt, kind="ExternalInput").ap()

    x = din("x", [S_LEN, D])
    mem = din("mem", [256, D])
    w_in = din("w_in", [D, 2560])
    w_out = din("w_out", [D, D])
    xa_q = din("xa_w_q", [D, D])
    xa_k = din("xa_w_k", [D, D])
    xa_v = din("xa_w_v", [D, D])
    xa_o = din("xa_w_o", [D, D])
    w_gate = din("w_gate", [16, D, 256])
    w_up = din("w_up", [16, D, 256])
    w_down = din("w_down", [16, 256, D])
    gains = din("gains", [5, 128, D])
    pp_d = din("pp", [128, 40])
    wbd_d = din("wbd", [128, 8, 128])
    wr_d = din("wr", [D, 20])
    rb_d = din("rb", [128, 20])
    y = nc.dram_tensor("y", [S_LEN, D], F32, kind="ExternalOutput").ap()
    skind = "ExternalOutput" if dbg else "Internal"
    x1s = nc.dram_tensor("x1s", [S_LEN, D], F32, kind=skind).ap()
    x2s = nc.dram_tensor("x2s", [S_LEN, D], F32, kind=skind).ap()
    dbg1 = nc.dram_tensor("dbg1", [128, 8, 512], BF16, kind=skind).ap()
    dbgB = Buf("dbg1")
    wg_s = nc.dram_tensor("wg_s", [16, 128, 8 * 256], BF16).ap()
    wu_s = nc.dram_tensor("wu_s", [16, 128, 8 * 256], BF16).ap()
    wd_s = nc.dram_tensor("wd_s", [16, 128, 2 * 1024], BF16).ap()

    _uid = [0]

    def _sbt(es_, n, s, d):
        _uid[0] += 1
        return es_.enter_context(nc.sbuf_tensor("%s_u%d" % (n, _uid[0]), s, d))

    def _pst(es_, n, s, d):
        _uid[0] += 1
        return es_.enter_context(nc.psum_tensor("%s_u%d" % (n, _uid[0]), s, d))

    with ExitStack() as eg:
        S = Sched(nc, eg)
        k = K(S)
        k_rec = k
        gsb = lambda n, s, d: _sbt(eg, n, s, d)
        ident = gsb("ident", [128, 128], BF16)
        negU = gsb("negU", [128, 128], BF16)
        negOnes = gsb("negOnes", [128, 128], BF16)
        ones = gsb("ones", [128, 128], BF16)
        maskneg = gsb("maskneg", [128, 128], BF16)
        maskfull = gsb("maskfull", [128, 512], BF16)
        sel = gsb("sel", [128, 16, 128], BF16)
        cst = gsb("cst", [128, 4], F32)
        pp = gsb("pp", [128, 40], F32)
        c12 = gsb("c12", [128, 20], F32)
        constB = Buf("const")
        ppB = Buf("pp")
        _wsB = Buf("wscr")
        wgsB = [_wsB for e in range(16)]
        wusB = [_wsB for e in range(16)]
        wdsB = [_wsB for e in range(16)]
        _x1B = [Buf("x1s%d" % i) for i in range(4)]
        _x2B = [Buf("x2s%d" % i) for i in range(4)]
        x1B = [_x1B[i % 4] for i in range(32)]
        x2B = [_x2B[i % 4] for i in range(32)]

        k.memset("pool", ident[:], 1.0, [constB])
        k.asel(ident[:], ident[:], [[-1, 128]], ALU.is_equal, 0.0, 0, 1, [constB], [constB])
        k.memset("pool", negU[:], -1.0, [constB])
        k.asel(negU[:], negU[:], [[-1, 128]], ALU.is_ge, 0.0, 0, 1, [constB], [constB])
        k.memset("pool", negOnes[:], -1.0, [constB])
        k.memset("pool", ones[:], 1.0, [constB])
        k.memset("pool", maskneg[:], NEG, [constB])
        k.asel(maskneg[:], maskneg[:], [[-1, 128]], ALU.is_ge, 0.0, 0, 1, [constB], [constB])
        k.memset("pool", maskfull[:], 0.0, [constB])
        k.copy("pool", maskfull[:, 0:128], maskneg[:], [constB], [constB])
        k.memset("pool", sel[:], 1.0, [constB])
        k.asel(sel[:], sel[:], [[-1, 16], [0, 128]], ALU.is_equal, 0.0, 0, 1, [constB], [constB])
        k.memset("pool", cst[:, 0:1], EPS, [constB])
        k.memset("pool", cst[:, 1:2], 1e-18, [constB])
        k.memset("pool", cst[:, 2:3], 1.0, [constB])
        S.dma("sp", pp[:], pp_d[:, :], writes=[ppB])
        k.act(c12[:, 0:4], pp[:, 28:32], AF.Exp, [ppB], [constB], scale=-1.0)
        k.act(c12[:, 0:4], c12[:, 0:4], AF.Ln, [constB], [constB], bias=cst[:, 2:3])
        k.ts("dve", c12[:, 4:8], c12[:, 0:4], -8.0, None, ALU.mult, None, [constB], [constB])
        k.ts("dve", c12[:, 8:12], c12[:, 0:4], -16.0, None, ALU.mult, None, [constB], [constB])
        moe_cvt = []
        for e in range(16):
            moe_cvt.append(lambda e=e: S.dma("pool", wg_s[e].rearrange("p (c f) -> p c f", c=8),
                                             w_gate[e].rearrange("(c p) f -> p c f", p=128), writes=[wgsB[e]],
                                             serialize=False))
            moe_cvt.append(lambda e=e: S.dma("pool", wu_s[e].rearrange("p (c f) -> p c f", c=8),
                                             w_up[e].rearrange("(c p) f -> p c f", p=128), writes=[wusB[e]],
                                             serialize=False))
            moe_cvt.append(lambda e=e: S.dma("pool", wd_s[e].rearrange("p (c f) -> p c f", c=2),
                                             w_down[e].rearrange("(c p) f -> p c f", p=128), writes=[wdsB[e]],
                                             serialize=False))

        G = dict(ident=ident, negU=negU, negOnes=negOnes, ones=ones, maskneg=maskneg, sel=sel, pp=pp,
                 c12=c12, constB=constB, ppB=ppB)

        def norm_T(es_tiles, xs, xsB, gain, gainB, dstT, dstB, j, evac_eng, kx=None, sq_dve=False):
            k = kx if kx is not None else k_rec
            ss, ssB = es_tiles["ss"].next()
            hn, hnB = es_tiles["hn"].next()
            pT, pTB = es_tiles["pT"].next()
            if sq_dve:
                k.stt(hn[:], xs, 1.0, xs, ALU.mult, ALU.mult, [xsB], [hnB, ssB], accum_out=ss[:, 0:1])
            else:
                k.act(hn[:], xs, AF.Square, [xsB], [hnB, ssB], accum_out=ss[:, 0:1])
            k.act(ss[:, 1:2], ss[:, 0:1], AF.Ln, [ssB, constB], [ssB], scale=1.0 / D, bias=cst[:, 0:1])
            k.act(ss[:, 2:3], ss[:, 1:2], AF.Exp, [ssB], [ssB], scale=-0.5)
            for h_ in range(2):
                k.stt(hn[:, h_ * 512:(h_ + 1) * 512], xs[:, h_ * 512:(h_ + 1) * 512], ss[:, 2:3],
                      gain[:, h_ * 512:(h_ + 1) * 512], ALU.mult, ALU.mult, [xsB, ssB, gainB], [hnB])
            for dc in range(8):
                k.tr(pT[:, dc * 128:(dc + 1) * 128], hn[:, dc * 128:(dc + 1) * 128], ident[:],
                     [hnB, constB], [pTB], inc=(dc == 7))
            k.copy(evac_eng, dstT[:, :, j * 128:(j + 1) * 128], pT[:].rearrange("p (c t) -> p c t", c=8),
                   [pTB], [dstB])
            return ss, ssB

        def mk_norm_tiles(es, nc_):
            sb = lambda n, s, d: _sbt(es, n, s, d)
            sst = [sb("ss%d" % i, [128, 4], F32) for i in range(3)]
            t = dict(
                ss=Rot(sst, "ss"),
                hn=Rot([sb("hn%d" % i, [128, D], BF16) for i in range(2)], "hn"),
                pT=Rot([_pst(es, "pT%d" % i, [128, 1024], BF16) for i in range(1)], "pT"),
            )
            return t

        with ExitStack() as es:
            sb = lambda n, s, d: _sbt(es, n, s, d)
            psb = lambda n: _pst(es, n, [128, 512], F32)
            wbd = sb("wbd", [128, 8, 128], BF16)
            gmix = sb("gmix", [128, D], F32)
            KT = sb("KT", [128, 4, S_LEN], BF16)
            V = sb("V", [128, 32, 512], BF16)
            hal = sb("hal", [128, 4, 4], F32)
            hst = sb("hst", [128, 4], F32)
            gg = sb("gg", [128, 4, 512], BF16)
            hT = sb("hT", [128, 8, 512], BF16)
            qT2 = [(sb("qT%d" % i, [128, 8, 512], BF16), Buf("qT%d" % i)) for i in range(2)]
            yT2 = [(sb("yT%d" % i, [128, 8, 512], BF16), Buf("yT%d" % i)) for i in range(2)]
            rstdF = sb("rstdF", [128, 512], F32)
            rstdK = rstdF
            winR = Rot([sb("win%d" % i, [128, 8, 256], BF16) for i in range(4)], "win")
            woR = Rot([sb("wo%d" % i, [128, 8, 512], BF16) for i in range(2)], "wo")
            xsR = Rot([sb("xs%d" % i, [128, D], F32) for i in range(2)], "xs")
            xhR = Rot([sb("xh%d" % i, [128, 512], F32) for i in range(2)], "xh")
            t512 = lambda n, d, c: Rot([sb("%s%d" % (n, i), [128, 512], d) for i in range(c)], n)
            LT = []
            for i in range(2):
                T = {"xrt": sb("xrt%d" % i, [128, 515], F32), "xcb": sb("xcb%d" % i, [128, 512], BF16)}
                for n_ in ("xc", "r_", "ig", "b_"):
                    T[n_] = sb("%s%d" % (n_, i), [128, 512], F32)
                for n_ in ("xrtB", "xcB", "rB", "igB", "bB", "xcbB"):
                    T[n_] = Buf("%s%d" % (n_, i))
                LT.append(T)
            eR = Rot([sb("e_%d" % i, [128, 512], F32) for i in range(2)], "e")
            halBs = [Buf("hal%d" % i) for i in range(4)]
            hstBs = [Buf("hst%d" % i) for i in range(4)]
            sqR = t512("sq", BF16, 2)
            spR = t512("sp", BF16, 3)
            wR = t512("w", BF16, 3)
            RR = t512("R", BF16, 2)
            pmR = Rot([psb("pm%d" % i) for i in range(2)], "pm")
            ZR = Rot([psb("Z%d" % i) for i in range(4)], "Z")
            OR = Rot([psb("O%d" % i) for i in range(1)], "O")
            nt = mk_norm_tiles(es, nc)
            wbdB, gmixB = Buf("wbd"), Buf("gmix")
            KTb = [Buf("KT%d" % i) for i in range(NT)]
            Vb = [Buf("V%d" % i) for i in range(NT)]
            halB, hstB, ggB, hTB, rstdFB = (Buf("hal"), Buf("hst"), Buf("gg"), Buf("hT"), Buf("rstdF"))
            rstdKB = rstdFB
            S.dma("pool", wbd[:], wbd_d[:, :, :], writes=[wbdB])
            S.dma("sp", gmix[:], gains[0], writes=[gmixB])
            k.memset("pool", hal[:], 0.0, halBs)
            k.memset("pool", hst[:], 0.0, hstBs)
            for i in range(2):
                k.memset("pool", qT2[i][0][:], 0.0, [qT2[i][1]])
            k.ts("dve", c12[:, 12:20], pp[:, 20:28], -1.0, None, ALU.mult, None, [ppB], [constB])

            blocks = [4, 5, 6, 7, 8, 9, 2, 3, 0, 1]
            wseq = [(qt_, b) for qt_ in range(NT) for b in blocks]
            wst = {"n": 0, "no": 0}
            wtiles, wotiles = {}, {}

            def get_w(kk, s):
                while wst["n"] < min(s + 3, len(wseq)):
                    i = wst["n"]
                    b = wseq[i][1]
                    t, tB = winR.next()
                    kk.dma("pool", t[:], w_in[:, b * 256:(b + 1) * 256].rearrange("(c p) f -> p c f", p=128),
                          writes=[tB])
                    wtiles[i] = (t, tB)
                    wst["n"] += 1
                return wtiles[s]

            def get_wo(kk, s):
                while wst["no"] < min(s + 2, 2 * NT):
                    i = wst["no"]
                    half = i % 2
                    t, tB = woR.next()
                    kk.dma("pool", t[:], w_out[:, half * 512:(half + 1) * 512].rearrange("(c p) f -> p c f", p=128),
                          writes=[tB])
                    wotiles[i] = (t, tB)
                    wst["no"] += 1
                return wotiles[s]

            def proj(kk, wt, wtB, lo):
                pa, paB = pmR.next()
                for dc in range(8):
                    kk.mm(pa[:, :], wt[:, dc, lo:lo + 128], hT[:, dc, :], dc == 0, dc == 7,
                         [wtB, hTB], [paB], inc=(dc == 7))
                return pa, paB

            def sigmoid_from(kk, dst, dstB, pa, paB, nbias, on_act=False):
                kk.act(dst[:], pa[:, :], AF.Exp, [paB, constB], [dstB], scale=-1.0, bias=nbias)
                if on_act:
                    kk.act(dst[:], dst[:], AF.Ln, [dstB, constB], [dstB], bias=cst[:, 2:3])
                    kk.act(dst[:], dst[:], AF.Exp, [dstB], [dstB], scale=-1.0)
                else:
                    kk.ts("dve", dst[:], dst[:], 1.0, None, ALU.add, None, [dstB], [dstB])
                    for q_ in range(4):
                        kk.recip(dst[:, q_ * 128:(q_ + 1) * 128], dst[:, q_ * 128:(q_ + 1) * 128], [dstB], [dstB])

            def lru_chain(kk, qt, c, wt, wtB, lo, T, bank, yTt, yTB):
                pa, paB = bank
                for dc in range(8):
                    kk.mm(pa[:, :], wt[:, dc, lo:lo + 128], hT[:, dc, :], dc == 0, dc == 7,
                          [wtB, hTB], [paB], inc=(dc == 7))
                kk.copy("dve", T["xrt"][:, 0:3], hal[:, c, 0:3], [halBs[c]], [T["xrtB"]])
                kk.copy("dve", T["xrt"][:, 3:515], pa[:, :], [paB], [T["xrtB"]])
                kk.ts("dve", T["xc"][:], T["xrt"][:, 0:512], pp[:, c * 4:c * 4 + 1], pp[:, 16 + c:17 + c],
                     ALU.mult, ALU.add, [T["xrtB"], ppB], [T["xcB"]])
                for t in range(1, 4):
                    kk.stt(T["xc"][:], T["xrt"][:, t:t + 512], pp[:, c * 4 + t:c * 4 + t + 1], T["xc"][:],
                          ALU.mult, ALU.add, [T["xrtB"], ppB, T["xcB"]], [T["xcB"]])
                kk.copy("dve", hal[:, c, 0:3], T["xrt"][:, 512:515], [T["xrtB"]], [halBs[c]])
                kk.copy("dve", T["xcb"][:], T["xc"][:], [T["xcB"]], [T["xcbB"]])
                pg, pgB = bank
                kk.mm(pg[:, :], wbd[:, c, :], T["xcb"][:], True, True, [wbdB, T["xcbB"]], [pgB], inc=True)
                sigmoid_from(kk, T["r_"], T["rB"], pg, pgB, c12[:, 12 + c:13 + c], on_act=(qt <= 3))
                pg2, pg2B = bank
                kk.mm(pg2[:, :], wbd[:, 4 + c, :], T["xcb"][:], True, True, [wbdB, T["xcbB"]], [pg2B], inc=True)
                sigmoid_from(kk, T["ig"], T["igB"], pg2, pg2B, c12[:, 16 + c:17 + c], on_act=(qt <= 3))
                kk.act(T["b_"][:], T["r_"][:], AF.Exp, [T["rB"], constB], [T["bB"]], scale=c12[:, 8 + c:9 + c])
                kk.act(T["r_"][:], T["r_"][:], AF.Exp, [T["rB"], constB], [T["rB"]], scale=c12[:, 4 + c:5 + c])
                kk.ts("dve", T["b_"][:], T["b_"][:], 0.99999994, -1.0, ALU.min, ALU.mult, [T["bB"]], [T["bB"]])
                kk.act(T["b_"][:], T["b_"][:], AF.Ln, [T["bB"], constB], [T["bB"]], bias=cst[:, 2:3])
                kk.act(T["b_"][:], T["b_"][:], AF.Exp, [T["bB"]], [T["bB"]], scale=0.5)
                kk.tt("dve", T["ig"][:], T["ig"][:], T["xc"][:], ALU.mult, [T["igB"], T["xcB"]], [T["igB"]])
                kk.tt("dve", T["b_"][:], T["b_"][:], T["ig"][:], ALU.mult, [T["bB"], T["igB"]], [T["bB"]])
                kk.scan(T["xc"][:, 0:256], T["r_"][:, 0:256], T["b_"][:, 0:256], hst[:, c:c + 1],
                        [T["rB"], T["bB"], hstBs[c], T["xcB"]], [T["xcB"]])
                kk.scan(T["xc"][:, 256:512], T["r_"][:, 256:512], T["b_"][:, 256:512], T["xc"][:, 255:256],
                        [T["rB"], T["bB"], T["xcB"]], [T["xcB"]])
                kk.copy("dve", hst[:, c:c + 1], T["xc"][:, 511:512], [T["xcB"]], [hstBs[c]])
                kk.tt("dve", yTt[:, c, :], T["xc"][:], gg[:, c, :], ALU.mult, [T["xcB"], ggB], [yTB])
                sq, sqB = sqR.next()
                kk.tt("dve", sq[:], yTt[:, c, :], yTt[:, c, :], ALU.mult, [yTB], [sqB])
                ps_, psB_ = bank
                kk.mm(ps_[:, :], ones[:], sq[:], True, True, [constB, sqB], [psB_], inc=True)
                if c == 0:
                    kk.copy("dve", rstdF[:], ps_[:, :], [psB_], [rstdFB])
                else:
                    kk.tt("dve", rstdF[:], ps_[:, :], rstdF[:], ALU.add, [psB_, rstdFB], [rstdFB])

            fsplit = {}

            def front_ops(qt):
                ops = []
                kk = Defer(k, S, ops)
                cur = qt % 2
                qTt, qTB = qT2[cur]
                yTt, yTB = yT2[cur]
                for j in range(4):
                    n = qt * 4 + j
                    xs, xsB = xsR.next()
                    kk.dma("sp", xs[:], x[n * 128:(n + 1) * 128, :], writes=[xsB])
                    norm_T(nt, xs[:], xsB, gmix[:], gmixB, hT, hTB, j, "dve", kx=kk, sq_dve=(qt >= 5))
                ssP = None
                for bi, b in enumerate(blocks):
                    if b == 2:
                        fsplit[qt] = len(ops)
                    wt, wtB = get_w(kk, qt * 10 + bi)
                    if b in (8, 9):
                        for j in range(4):
                            pa, paB = pmR.next()
                            for dc in range(8):
                                kk.mm(pa[:, 0:256], hT[:, dc, j * 128:(j + 1) * 128], wt[:, dc, :], dc == 0, dc == 7,
                                     [wtB, hTB], [paB], inc=(dc == 7))
                            kk.copy("dve", V[:, qt * 4 + j, (b - 8) * 256:(b - 7) * 256], pa[:, 0:256], [paB], [Vb[qt]])
                        continue
                    if b in (0, 1):
                        chains = []
                        for half in range(2):
                            cops = []
                            lru_chain(Defer(k, S, cops), qt, b * 2 + half, wt, wtB, half * 128, LT[half],
                                      (pmR.t[half], pmR.b[half]), yTt, yTB)
                            chains.append(cops)
                        for i in range(max(len(chains[0]), len(chains[1]))):
                            for cops in chains:
                                if i < len(cops):
                                    ops.append(cops[i])
                        continue
                    for half in range(2):
                        pa, paB = proj(kk, wt, wtB, half * 128)
                        if b in (4, 5):
                            c = (b - 4) * 2 + half
                            kk.ts("dve", qTt[0:64, 2 * c, :], pa[0:64, :], 0.125, None, ALU.mult, None, [paB], [qTB])
                            kk.ts("dve", qTt[64:128, 2 * c + 1, :], pa[64:128, :], 0.125, None, ALU.mult, None,
                                 [paB], [qTB])
                        elif b in (6, 7):
                            c = (b - 6) * 2 + half
                            kk.copy("dve", KT[:, c, qt * 512:(qt + 1) * 512], pa[:, :], [paB], [KTb[qt]])
                        elif b in (2, 3):
                            c = (b - 2) * 2 + half
                            kk.copy("dve", gg[:, c, :], pa[:, :], [paB], [ggB])
                            if c == 3:
                                def _gelu4():
                                    for c_ in range(4):
                                        k_rec.act(gg[:, c_, :], gg[:, c_, :], AF.Gelu_apprx_tanh, [ggB], [ggB])
                                ops.append(_gelu4)
                kk.act(rstdF[:], rstdF[:], AF.Ln, [rstdFB, constB], [rstdFB], scale=1.0 / 512, bias=cst[:, 0:1])
                kk.act(rstdF[:], rstdF[:], AF.Exp, [rstdFB], [rstdFB], scale=-0.5)
                for c in range(4):
                    kk.stt(yTt[:, c, :], yTt[:, c, :], pp[:, 32 + c:33 + c], rstdF[:], ALU.mult, ALU.mult,
                          [yTB, ppB, rstdFB], [yTB])
                return ops

            def back_ops(qt):
                ops = []
                kk = Defer(k, S, ops)
                yTt, yTB = yT2[qt % 2]
                ssP, ssPB = pmR.next()
                for pr in range(4):
                    sq, sqB = sqR.next()
                    kk.tt("dve", sq[:], yTt[:, 4 + pr, :], yTt[:, 4 + pr, :], ALU.mult, [yTB], [sqB])
                    kk.mm(ssP[:, :], ones[:], sq[:], pr == 0, pr == 3, [constB, sqB], [ssPB], inc=True)
                kk.act(rstdK[:], ssP[:, :], AF.Ln, [ssPB, constB], [rstdKB], scale=1.0 / 512, bias=cst[:, 0:1])
                kk.act(rstdK[:], rstdK[:], AF.Exp, [rstdKB], [rstdKB], scale=-0.5)
                for pr in range(4):
                    kk.stt(yTt[:, 4 + pr, :], yTt[:, 4 + pr, :], pp[:, 36 + pr:37 + pr], rstdK[:], ALU.mult, ALU.mult,
                          [yTB, ppB, rstdKB], [yTB])
                if dbg and qt == 0:
                    kk.dma("sp", dbg1[:, :, :], yTt[:], reads=[yTB], writes=[dbgB])
                for half in range(2):
                    wo_, woB_ = get_wo(kk, qt * 2 + half)
                    for j in range(4):
                        n = qt * 4 + j
                        xh, xhB = xhR.next()
                        kk.dma("sp", xh[:], x[n * 128:(n + 1) * 128, half * 512:(half + 1) * 512], writes=[xhB])
                        pa, paB = pmR.next()
                        for c in range(8):
                            kk.mm(pa[:, :], yTt[:, c, j * 128:(j + 1) * 128], wo_[:, c, :], c == 0, c == 7,
                                 [yTB, woB_], [paB], inc=(c == 7))
                        kk.tt("dve", xh[:], pa[:, :], xh[:], ALU.add, [paB, xhB], [xhB])
                        kk.dma("sp", x1s[n * 128:(n + 1) * 128, half * 512:(half + 1) * 512], xh[:], reads=[xhB],
                              writes=[x1B[n]])
                return ops

            def attention(qt, side):
                qTt, qTB = qT2[qt % 2]
                yTt, yTB = yT2[qt % 2]
                items = []
                for h in range(8):
                    kmax = 4 * qt + 3
                    for kb in range(kmax, -1, -1):
                        jd = kb - 4 * qt
                        c0 = jd * 128 if jd > 0 else 0
                        items.append(dict(h=h, kb=kb, c0=c0, diag=(jd >= 0), first=(kb == kmax), last=(kb == 0)))
                n_it = len(items)
                st = {}

                def s0(i):
                    it = items[i]
                    h, kb, c0 = it["h"], it["kb"], it["c0"]
                    Z, ZB = ZR.next()
                    it["Z"], it["ZB"] = Z, ZB
                    k.mm(Z[:, c0:512], KT[:, h // 2, kb * 128:(kb + 1) * 128], qTt[:, h, c0:512],
                         True, not it["diag"], [KTb[kb // 4], qTB], [ZB], inc=(not it["diag"]))
                    if it["diag"]:
                        k.mm(Z[:, c0:512], ident[:], maskfull[:, 0:512 - c0], False, True, [constB], [ZB], inc=True)
                    if it["first"]:
                        R, RB = RR.next()
                        st[("R", h)] = (R, RB)
                        k.memset("dve", R[:], 0.0, [RB])

                def s1a(i):
                    it = items[i]
                    c0 = it["c0"]
                    e_, eB = eR.next()
                    it["e"], it["eB"] = e_, eB
                    k.act(e_[:, c0:512], it["Z"][:, c0:512], AF.Exp, [it["ZB"]], [eB])

                def s1b(i):
                    it = items[i]
                    c0 = it["c0"]
                    sp, spB = spR.next()
                    it["sp"], it["spB"] = sp, spB
                    k.act(sp[:, c0:512], it["e"][:, c0:512], AF.Ln, [it["eB"], constB], [spB], bias=cst[:, 2:3])

                def s3(i):
                    it = items[i]
                    c0 = it["c0"]
                    R, RB = st[("R", it["h"])]
                    Z, ZB = it["Z"], it["ZB"]
                    k.mm(Z[:, c0:512], negU[:], it["sp"][:, c0:512], False, it["first"], [constB, it["spB"]], [ZB],
                         inc=it["first"], skip=True)
                    if not it["first"]:
                        k.mm(Z[:, c0:512], negOnes[:], R[:, c0:512], False, True, [constB, RB], [ZB], inc=True,
                             skip=True)
                    if not it["last"]:
                        k.tt("dve", R[:, c0:512], R[:, c0:512], it["sp"][:, c0:512], ALU.add, [RB, it["spB"]], [RB])

                def s4(i):
                    it = items[i]
                    c0 = it["c0"]
                    w, wB = wR.next()
                    it["w"], it["wB"] = w, wB
                    k.act(w[:, c0:512], it["Z"][:, c0:512], AF.Exp, [it["ZB"]], [wB])

                def s5(i):
                    it = items[i]
                    h, kb, c0 = it["h"], it["kb"], it["c0"]
                    pr, hp = h // 2, (h % 2) * 64
                    if it["first"]:
                        st["O"] = OR.next()
                    O, OB = st["O"]
                    k.mm(O[:, c0:512], V[:, kb, pr * 128:(pr + 1) * 128], it["w"][:, c0:512],
                         it["first"], it["last"], [Vb[kb // 4], it["wB"]], [OB], inc=it["last"], skip=True)
                    if it["last"]:
                        k.copy("dve", yTt[hp:hp + 64, 4 + pr, :], O[hp:hp + 64, :], [OB], [yTB])

                steps = n_it + 4
                n_side = len(side)
                done = 0
                for ti, t in enumerate(range(-3, n_it + 1)):
                    if 0 <= t + 3 < n_it:
                        s0(t + 3)
                    if 0 <= t + 1 < n_it:
                        s1b(t + 1)
                    if 0 <= t + 2 < n_it:
                        s1a(t + 2)
                    if 0 <= t < n_it:
                        s3(t)
                        s4(t)
                    if 0 <= t - 1 < n_it:
                        s5(t - 1)
                    want = (n_side * (ti + 1)) // steps
                    while done < want:
                        side[done]()
                        done += 1
                while done < n_side:
                    side[done]()
                    done += 1

            f0 = front_ops(0)
            for f in f0[:fsplit[0]]:
                f()
            for qt in range(NT):
                side = []
                if qt == 0:
                    side += f0[fsplit[0]:]
                if qt >= 1:
                    side += back_ops(qt - 1)
                if qt + 1 < NT:
                    side += front_ops(qt + 1)
                side += moe_cvt[qt * 6:(qt + 1) * 6]
                attention(qt, side)
            for f in back_ops(NT - 1):
                f()
            S.barrier()
            S.emit()
        if stop_after == 1:
            return nc

        with ExitStack() as es:
            sb = lambda n, s, d: _sbt(es, n, s, d)
            psb = lambda n: _pst(es, n, [128, 512], F32)
            wq = sb("wq", [128, 8, D], BF16)
            wo = sb("wo", [128, 8, D], BF16)
            gxa = sb("gxa", [128, D], F32)
            kmT = sb("kmT", [128, 8, 256], BF16)
            vm = sb("vm", [128, 2, D], BF16)
            qT2R = Rot([sb("qT2_%d" % i, [128, 8, 512], BF16) for i in range(2)], "qT2")
            pTsR = Rot([sb("pT_sb%d" % i, [128, 8, 512], BF16) for i in range(2)], "pTs")
            oTR = Rot([sb("oT%d" % i, [128, 8, 512], BF16) for i in range(2)], "oT")
            hT = Rot([sb("h2T%d" % i, [128, 8, 512], BF16) for i in range(2)], "h2T")
            xrR = Rot([sb("xr%d" % i, [128, D], F32) for i in range(2)], "xr")
            xsR = Rot([sb("xs%d" % i, [128, D], F32) for i in range(2)], "xs")
            pexR = Rot([sb("pex%d" % i, [128, 4, 256], F32) for i in range(2)], "pex")
            pnR = Rot([sb("pn%d" % i, [128, 4, 256], BF16) for i in range(2)], "pn")
            smR = Rot([sb("sm%d" % i, [128, 16], F32) for i in range(2)], "sm")
            pmR = Rot([psb("pm%d" % i) for i in range(2)], "pm")
            pqR = Rot([psb("pq%d" % i) for i in range(2)], "pq")
            scR = Rot([psb("sc%d" % i) for i in range(2)], "sc")
            ptR = Rot([_pst(es, "pt2_%d" % i, [128, 1024], BF16) for i in range(1)], "pt2")
            nt = mk_norm_tiles(es, nc)
            wqB, woB, gxaB, kmTB, vmB = (Buf("wq"), Buf("wo"), Buf("gxa"), Buf("kmT"), Buf("vm"))
            S.dma("pool", wq[:], xa_q.rearrange("(c p) f -> p c f", p=128), writes=[wqB])
            S.dma("pool", wo[:], xa_o.rearrange("(c p) f -> p c f", p=128), writes=[woB])
            S.dma("sp", gxa[:], gains[1], writes=[gxaB])
            with ExitStack() as es0:
                sb0 = lambda n, s, d: _sbt(es0, n, s, d)
                wk = sb0("wk", [128, 8, D], BF16)
                wv = sb0("wv", [128, 8, D], BF16)
                gm = sb0("gm", [128, D], F32)
                mT = sb0("mT", [128, 8, 512], BF16)
                wkB, wvB, gmB, mTB = Buf("wk"), Buf("wv"), Buf("gm"), Buf("mT")
                S.dma("pool", wk[:], xa_k.rearrange("(c p) f -> p c f", p=128), writes=[wkB])
                S.dma("pool", wv[:], xa_v.rearrange("(c p) f -> p c f", p=128), writes=[wvB])
                S.dma("sp", gm[:], gains[4], writes=[gmB])
                for j in range(2):
                    xs, xsB = xsR.next()
                    S.dma("sp", xs[:], mem[j * 128:(j + 1) * 128, :], writes=[xsB])
                    norm_T(nt, xs[:], xsB, gm[:], gmB, mT, mTB, j, "dve")
                for fc in range(8):
                    pa, paB = pmR.next()
                    for dc in range(8):
                        k.mm(pa[:, 0:256], wk[:, dc, fc * 128:(fc + 1) * 128], mT[:, dc, 0:256], dc == 0, dc == 7,
                             [wkB, mTB], [paB], inc=(dc == 7))
                    k.copy("act", kmT[:, fc, :], pa[:, 0:256], [paB], [kmTB])
                for mc in range(2):
                    for half in range(2):
                        pa, paB = pmR.next()
                        for dc in range(8):
                            k.mm(pa[:, :], mT[:, dc, mc * 128:(mc + 1) * 128], wv[:, dc, half * 512:(half + 1) * 512],
                                 dc == 0, dc == 7, [wvB, mTB], [paB], inc=(dc == 7))
                        k.copy("dve", vm[:, mc, half * 512:(half + 1) * 512], pa[:, :], [paB], [vmB])
                S.barrier()
                S.emit()
            st2 = {}

            def F2(qt):
                ops = []
                kk = Defer(k, S, ops)
                hTt, hTB = hT.next()
                qT2, qT2B = qT2R.next()
                st2[qt] = (qT2, qT2B)
                for j in range(4):
                    n = qt * 4 + j
                    xs, xsB = xsR.next()
                    kk.dma("sp", xs[:], x1s[n * 128:(n + 1) * 128, :], reads=[x1B[n]], writes=[xsB])
                    norm_T(nt, xs[:], xsB, gxa[:], gxaB, hTt, hTB, j, "dve", kx=kk)
                for fc in range(8):
                    pa, paB = pqR.next()
                    for dc in range(8):
                        kk.mm(pa[:, :], wq[:, dc, fc * 128:(fc + 1) * 128], hTt[:, dc, :], dc == 0, dc == 7,
                             [wqB, hTB], [paB], inc=(dc == 7))
                    if fc % 2 == 0:
                        kk.act(qT2[:, fc, :], pa[:, :], AF.Copy, [paB], [qT2B], scale=0.0625)
                    else:
                        kk.ts("dve", qT2[:, fc, :], pa[:, :], 0.0625, None, ALU.mult, None, [paB], [qT2B])
                return ops

            def MB2(qt):
                ops = []
                kk = Defer(k, S, ops)
                qT2, qT2B = st2[qt]
                pT_sb, pTsB = pTsR.next()
                oT, oTB = oTR.next()
                for j in range(4):
                    pex, pexB = pexR.next()
                    pn, pnB = pnR.next()
                    sm, smB = smR.next()
                    for hp2 in range(2):
                        sc, scB = scR.next()
                        for hh in (2 * hp2, 2 * hp2 + 1):
                            for k2 in range(2):
                                fc = hh * 2 + k2
                                kk.mm(sc[:, (hh % 2) * 256:(hh % 2 + 1) * 256], qT2[:, fc, j * 128:(j + 1) * 128],
                                     kmT[:, fc, :], k2 == 0, k2 == 1, [qT2B, kmTB], [scB],
                                     inc=(k2 == 1 and hh % 2 == 1), skip=True)
                        kk.rmax(sm[:, 2 * hp2:2 * hp2 + 2], sc[:].rearrange("p (h m) -> p h m", h=2), [scB], [smB])
                        kk.ts("dve", sm[:, 4 + 2 * hp2:6 + 2 * hp2], sm[:, 2 * hp2:2 * hp2 + 2], -1.0, None, ALU.mult,
                             None, [smB], [smB])
                        for hh in (2 * hp2, 2 * hp2 + 1):
                            kk.act(pex[:, hh, :], sc[:, (hh % 2) * 256:(hh % 2 + 1) * 256], AF.Exp, [scB, smB],
                                  [pexB, smB], bias=sm[:, 4 + hh:5 + hh], accum_out=sm[:, 8 + hh:9 + hh])
                    kk.recip(sm[:, 12:16], sm[:, 8:12], [smB], [smB])
                    for hh in range(4):
                        kk.ts("dve", pn[:, hh, :], pex[:, hh, :], sm[:, 12 + hh:13 + hh], None, ALU.mult, None,
                             [pexB, smB], [pnB])
                    pt, ptB = ptR.next()
                    for hh in range(4):
                        for mc in range(2):
                            q8 = hh * 2 + mc
                            kk.tr(pt[:, q8 * 128:(q8 + 1) * 128], pn[:, hh, mc * 128:(mc + 1) * 128], ident[:],
                                 [pnB, constB], [ptB], inc=(q8 == 7))
                    kk.copy("act", pT_sb[:, :, j * 128:(j + 1) * 128], pt[:].rearrange("p (c t) -> p c t", c=8),
                           [ptB], [pTsB])
                for fc in range(8):
                    hh = fc // 2
                    pa, paB = pmR.next()
                    for mc in range(2):
                        kk.mm(pa[:, :], vm[:, mc, fc * 128:(fc + 1) * 128], pT_sb[:, hh * 2 + mc, :], mc == 0, mc == 1,
                             [vmB, pTsB], [paB], inc=(mc == 1))
                    kk.copy("act" if fc % 2 == 0 else "dve", oT[:, fc, :], pa[:, :], [paB], [oTB])
                for j in range(4):
                    n = qt * 4 + j
                    xs, xsB = xrR.next()
                    kk.dma("sp", xs[:], x1s[n * 128:(n + 1) * 128, :], reads=[x1B[n]], writes=[xsB])
                    for half in range(2):
                        pa, paB = pmR.next()
                        for c in range(8):
                            kk.mm(pa[:, :], oT[:, c, j * 128:(j + 1) * 128], wo[:, c, half * 512:(half + 1) * 512],
                                 c == 0, c == 7, [oTB, woB], [paB], inc=(c == 7))
                        kk.tt("dve", xs[:, half * 512:(half + 1) * 512], pa[:, :], xs[:, half * 512:(half + 1) * 512],
                             ALU.add, [paB, xsB], [xsB])
                    kk.dma("sp", x2s[n * 128:(n + 1) * 128, :], xs[:], reads=[xsB], writes=[x2B[n]])
                return ops

            for f in F2(0):
                f()
            for qt in range(NT):
                run_merged(MB2(qt), F2(qt + 1) if qt + 1 < NT else [])
            S.barrier()
            S.emit()
        if stop_after == 2:
            return nc

        with ExitStack() as es:
            sb = lambda n, s, d: _sbt(es, n, s, d)
            psb = lambda n: _pst(es, n, [128, 512], F32)
            gmoe = sb("gmoe", [128, D], F32)
            gfin = sb("gfin", [128, D], F32)
            wr = sb("wr", [128, 8, 20], BF16)
            rb = sb("rb", [128, 20], F32)
            tT = Rot([sb("tT%d" % i, [128, 8, 512], BF16) for i in range(2)], "tT")
            accR = Rot([sb("acc%d" % i, [128, 4, D], F32) for i in range(2)], "acc")
            hidR = Rot([sb("hid%d" % i, [128, 16, 512], BF16) for i in range(2)], "hid")
            wgR = Rot([sb("wg%d" % i, [128, 8, 256], BF16) for i in range(4)], "wg")
            wuR = Rot([sb("wu%d" % i, [128, 8, 256], BF16) for i in range(4)], "wu")
            wdR = Rot([sb("wd%d" % i, [128, 2, D], BF16) for i in range(10)], "wd")
            combT = Rot([sb("combT%d" % i, [128, 512], BF16) for i in range(2)], "combT")
            cbR = Rot([sb("cb%d" % i, [128, 512], BF16) for i in range(2)], "cb")
            sgR = Rot([sb("sg%d" % i, [128, 512], F32) for i in range(4)], "sg")
            t1R = Rot([sb("t1_%d" % i, [128, 512], F32) for i in range(3)], "t1")
            xsR = Rot([sb("xs%d" % i, [128, D], F32) for i in range(2)], "xs")
            yoR = Rot([sb("yo%d" % i, [128, D], F32) for i in range(2)], "yo")
            rtR = Rot([sb("rt%d" % i, [128, 96], F32) for i in range(2)], "rt")
            cmbR = Rot([sb("cmb%d" % i, [128, 128], BF16) for i in range(2)], "cmb")
            for i in range(2):
                k.memset("pool", cmbR.t[i][:], 0.0, [cmbR.b[i]])
            gR = Rot([psb("g%d" % i) for i in range(2)], "g")
            uR = Rot([psb("u%d" % i) for i in range(2)], "u")
            dnR = Rot([psb("dn%d" % i) for i in range(2)], "dn")
            rtP = Rot([psb("rtp%d" % i) for i in range(1)], "rtp")
            nt = mk_norm_tiles(es, nc)
            gmoeB, gfinB, wrB, rbB = Buf("gmoe"), Buf("gfin"), Buf("wr"), Buf("rb")
            yB = [Buf("y%d" % i) for i in range(4)]
            S.dma("sp", gmoe[:], gains[2], writes=[gmoeB])
            S.dma("sp", gfin[:], gains[3], writes=[gfinB])
            S.dma("pool", wr[:], wr_d.rearrange("(c p) f -> p c f", p=128), writes=[wrB])
            S.dma("sp", rb[:], rb_d[:, :], writes=[rbB])
            st3 = {}

            def front3(qt):
                ops = []
                kk = Defer(k, S, ops)
                tTt, tTB = tT.next()
                cT, cTB = combT.next()
                acc, accB = accR.next()
                st3[qt] = (tTt, tTB, cT, cTB, acc, accB)
                for j in range(4):
                    n = qt * 4 + j
                    xs, xsB = xsR.next()
                    kk.dma("sp", xs[:], x2s[n * 128:(n + 1) * 128, :], reads=[x2B[n]], writes=[xsB])
                    norm_T(nt, xs[:], xsB, gmoe[:], gmoeB, tTt, tTB, j, "dve", kx=kk)
                    for h_ in range(2):
                        kk.copy("dve", acc[:, j, h_ * 512:(h_ + 1) * 512], xs[:, h_ * 512:(h_ + 1) * 512], [xsB], [accB])
                    pm, pmB = rtP.next()
                    for dc in range(8):
                        kk.mm(pm[:, 0:20], tTt[:, dc, j * 128:(j + 1) * 128], wr[:, dc, :], dc == 0, dc == 7,
                             [tTB, wrB], [pmB], inc=(dc == 7))
                    rt, rtB = rtR.next()
                    cmb, cmbB = cmbR.next()
                    RW = dict(reads=[rtB], writes=[rtB])
                    lg = rt[:, 0:20]
                    kk.tt("dve", lg, pm[:, 0:20], rb[:, :], ALU.add, [pmB, rbB], [rtB])
                    gmax, ngmax, gmask, gexp, gsum, gp = (rt[:, 20:21], rt[:, 21:22], rt[:, 24:28], rt[:, 28:32],
                                                          rt[:, 22:23], rt[:, 23:24])
                    kk.rmax(gmax, rt[:, 0:4], **RW)
                    kk.ts("dve", ngmax, gmax, -1.0, None, ALU.mult, None, **RW)
                    kk.ts("dve", gmask, rt[:, 0:4], gmax, None, ALU.is_equal, None, **RW)
                    kk.act(gexp, rt[:, 0:4], AF.Exp, [rtB], [rtB], bias=ngmax, accum_out=gsum)
                    kk.recip(gp, gsum, **RW)
                    ch = rt[:, 32:36]
                    kk.ts("dve", ch, rt[:, 4:8], rt[:, 24:25], None, ALU.mult, None, **RW)
                    for g in range(1, 4):
                        kk.stt(ch, rt[:, 4 + 4 * g:8 + 4 * g], rt[:, 24 + g:25 + g], ch, ALU.mult, ALU.add, **RW)
                    m1, m2, mask1, c2, mask2 = rt[:, 36:37], rt[:, 37:38], rt[:, 40:44], rt[:, 44:48], rt[:, 48:52]
                    kk.rmax(m1, ch, **RW)
                    kk.ts("dve", mask1, ch, m1, None, ALU.is_equal, None, **RW)
                    kk.stt(c2, mask1, -1e30, ch, ALU.mult, ALU.add, **RW)
                    kk.rmax(m2, c2, **RW)
                    kk.ts("dve", mask2, c2, m2, None, ALU.is_equal, None, **RW)
                    dd, ed, w1, w2 = rt[:, 38:39], rt[:, 39:40], rt[:, 52:53], rt[:, 53:54]
                    kk.tt("dve", dd, m2, m1, ALU.subtract, **RW)
                    kk.act(ed, dd, AF.Exp, [rtB], [rtB])
                    kk.ts("dve", ed, ed, 1.0, None, ALU.add, None, **RW)
                    kk.recip(w1, ed, **RW)
                    kk.tt("dve", w1, w1, gp, ALU.mult, **RW)
                    kk.tt("dve", w2, gp, w1, ALU.subtract, **RW)
                    ew = rt[:, 56:60]
                    kk.ts("dve", ew, mask1, w1, None, ALU.mult, None, **RW)
                    kk.stt(ew, mask2, w2, ew, ALU.mult, ALU.add, **RW)
                    for g in range(4):
                        kk.ts("dve", cmb[:, 4 * g:4 * g + 4], ew, rt[:, 24 + g:25 + g], None, ALU.mult, None,
                             [rtB], [cmbB])
                    pt, ptB = nt["pT"].next()
                    kk.tr(pt[:, 0:128], cmb[:, :], ident[:], [cmbB, constB], [ptB], inc=True)
                    kk.copy("dve", cT[:, j * 128:(j + 1) * 128], pt[:, 0:128], [ptB], [cTB])
                return ops

            def experts3(qt, grp):
                ops = []
                kk = Defer(k, S, ops)
                tTt, tTB, cT, cTB, acc, accB = st3[qt]
                if True:
                    hid, hidB = hidR.next()
                    wds = []
                    for e8 in range(8):
                        E = grp * 8 + e8
                        wg, wgB = wgR.next()
                        wu, wuB = wuR.next()
                        wd, wdB = wdR.next()
                        wds.append((wd, wdB))
                        kk.dma("sp", wg[:], wg_s[E].rearrange("p (c f) -> p c f", c=8), reads=[wgsB[E]], writes=[wgB])
                        kk.dma("sp", wu[:], wu_s[E].rearrange("p (c f) -> p c f", c=8), reads=[wusB[E]], writes=[wuB])
                        kk.dma("sp", wd[:], wd_s[E].rearrange("p (c f) -> p c f", c=2), reads=[wdsB[E]], writes=[wdB])
                        cbp, cbpB = gR.next()
                        kk.mm(cbp[:, :], sel[:, E, :], cT[:, :], True, True, [constB, cTB], [cbpB], inc=True)
                        cb, cbB = cbR.next()
                        kk.copy("dve", cb[:], cbp[:, :], [cbpB], [cbB])
                        for fc in range(2):
                            gp_, gpB = gR.next()
                            up_, upB = uR.next()
                            for dc in range(8):
                                kk.mm(gp_[:, :], wg[:, dc, fc * 128:(fc + 1) * 128], tTt[:, dc, :], dc == 0, dc == 7,
                                     [wgB, tTB], [gpB], inc=(dc == 7))
                            for dc in range(8):
                                kk.mm(up_[:, :], wu[:, dc, fc * 128:(fc + 1) * 128], tTt[:, dc, :], dc == 0, dc == 7,
                                     [wuB, tTB], [upB], inc=(dc == 7))
                            sg, sgB = sgR.next()
                            kk.act(sg[:], gp_[:, :], AF.Silu, [gpB], [sgB])
                            t1, t1B = t1R.next()
                            kk.tt("dve", t1[:], up_[:, :], cb[:], ALU.mult, [upB, cbB], [t1B])
                            kk.tt("dve", hid[:, e8 * 2 + fc, :], t1[:], sg[:], ALU.mult, [t1B, sgB], [hidB])
                    for j in range(4):
                        for half in range(2):
                            dn, dnB = dnR.next()
                            for k3 in range(16):
                                wd, wdB = wds[k3 // 2]
                                kk.mm(dn[:, :], hid[:, k3, j * 128:(j + 1) * 128],
                                      wd[:, k3 % 2, half * 512:(half + 1) * 512], k3 == 0, k3 == 15,
                                      [hidB, wdB], [dnB], inc=(k3 == 15))
                            kk.tt("dve", acc[:, j, half * 512:(half + 1) * 512], dn[:, :],
                                 acc[:, j, half * 512:(half + 1) * 512], ALU.add, [dnB, accB], [accB])
                return ops

            def final3(qt):
                ops = []
                kk = Defer(k, S, ops)
                tTt, tTB, cT, cTB, acc, accB = st3[qt]
                for j in range(4):
                    n = qt * 4 + j
                    ss, ssB = nt["ss"].next()
                    yo, yoB = yoR.next()
                    hn, hnB = nt["hn"].next()
                    kk.act(hn[:], acc[:, j, :], AF.Square, [accB], [hnB, ssB], accum_out=ss[:, 0:1])
                    kk.act(ss[:, 1:2], ss[:, 0:1], AF.Ln, [ssB, constB], [ssB], scale=1.0 / D, bias=cst[:, 0:1])
                    kk.act(ss[:, 2:3], ss[:, 1:2], AF.Exp, [ssB], [ssB], scale=-0.5)
                    kk.stt(yo[:], acc[:, j, :], ss[:, 2:3], gfin[:], ALU.mult, ALU.mult, [accB, ssB, gfinB], [yoB])
                    kk.dma("sp", y[n * 128:(n + 1) * 128, :], yo[:], reads=[yoB], writes=[yB[n % 4]])
                return ops

            for f in front3(0):
                f()
            for qt in range(NT):
                side = []
                if qt >= 1:
                    side += final3(qt - 1)
                if qt + 1 < NT:
                    side += front3(qt + 1)
                run_merged(experts3(qt, 0) + experts3(qt, 1), side)
            for f in final3(NT - 1):
                f()
            S.barrier()
            S.emit()
    return nc


def make_in_maps(inputs):
    f = lambda a: np.ascontiguousarray(np.asarray(a, dtype=np.float32))
    L = 0
    x = f(inputs["x"])
    mem = f(inputs["mem"])
    rep = lambda v: np.broadcast_to(f(v).reshape(1, D), (128, D))
    gains = np.ascontiguousarray(np.stack([rep(inputs["norm_mix"][L]), rep(inputs["norm_xattn"][L]),
                                           rep(inputs["norm_moe"][L]), rep(inputs["norm_final"]),
                                           rep(inputs["norm_mem"][L])], 0))
    pc = lambda v: f(v).reshape(-1, 128).T
    pp = np.zeros((128, 40), np.float32)
    cw = f(inputs["conv_w"][L])
    for c in range(4):
        for t in range(4):
            pp[:, c * 4 + t] = cw[t, c * 128:(c + 1) * 128]
    pp[:, 16:20] = pc(inputs["conv_b"][L])
    pp[:, 20:24] = pc(inputs["lru_b_a"][L].reshape(-1))
    pp[:, 24:28] = pc(inputs["lru_b_x"][L].reshape(-1))
    pp[:, 28:32] = pc(inputs["lru_lambda"][L])
    pp[:, 32:36] = pc(inputs["norm_rnn_out"][L])
    pp[:, 36:40] = pc(inputs["norm_sb_out"][L])
    wa = f(inputs["lru_w_a"][L])
    wx = f(inputs["lru_w_x"][L])
    wbd = np.zeros((128, 8, 128), np.float32)
    for c in range(4):
        for hp in range(2):
            wbd[hp * 64:(hp + 1) * 64, c, hp * 64:(hp + 1) * 64] = wa[2 * c + hp]
            wbd[hp * 64:(hp + 1) * 64, 4 + c, hp * 64:(hp + 1) * 64] = wx[2 * c + hp]
    wr = np.zeros((D, 20), np.float32)
    wr[:, 0:4] = f(inputs["w_group_router"][L])
    wer = f(inputs["w_expert_router"][L])
    for g in range(4):
        wr[:, 4 + 4 * g:8 + 4 * g] = wer[g]
    rbv = np.concatenate([f(inputs["b_group_router"][L]).reshape(-1), f(inputs["b_expert_router"][L]).reshape(-1)])
    rb = np.ascontiguousarray(np.broadcast_to(rbv.reshape(1, 20), (128, 20)))
    shared = {
        "w_in": f(inputs["w_in"][L]), "w_out": f(inputs["w_out"][L]),
        "xa_w_q": f(inputs["xa_w_q"][L]), "xa_w_k": f(inputs["xa_w_k"][L]),
        "xa_w_v": f(inputs["xa_w_v"][L]), "xa_w_o": f(inputs["xa_w_o"][L]),
        "w_gate": f(inputs["w_gate"][L]).reshape(16, D, 256), "w_up": f(inputs["w_up"][L]).reshape(16, D, 256),
        "w_down": f(inputs["w_down"][L]).reshape(16, 256, D),
        "gains": gains, "pp": pp, "wbd": wbd, "wr": wr, "rb": rb,
    }
    return [dict(shared, x=x[b], mem=mem[b]) for b in range(x.shape[0])]


_NC_CACHE = {}


def kernel(**inputs):
    in_maps = make_in_maps(inputs)
    if "nc" not in _NC_CACHE:
        _NC_CACHE["nc"] = build()
    nc = _NC_CACHE["nc"]
    res = run_bass_kernel_spmd(nc, in_maps, core_ids=list(range(8)))
    out = np.stack([np.asarray(r["y"], dtype=np.float32) for r in res.results], 0)
    return out
```

```python
import numpy as np
import concourse.bass as bass
import concourse.mybir as mybir
from concourse.bass_utils import run_bass_kernel_spmd
from contextlib import ExitStack

F32 = mybir.dt.float32
BF16 = mybir.dt.bfloat16
AF = mybir.ActivationFunctionType
ALU = mybir.AluOpType
AX = mybir.AxisListType

S_LEN = 4096
D = 1024
NT = 8
EPS = 1e-6
NEG = -30000.0


class Buf:
    __slots__ = ("name", "w", "r", "dsem")

    def __init__(self, name):
        self.name = name
        self.w = None
        self.r = {}
        self.dsem = None


class Rot:
    def __init__(self, tiles, name):
        self.t = tiles
        self.b = [Buf("%s%d" % (name, i)) for i in range(len(tiles))]
        self.i = 0

    def next(self):
        k = self.i % len(self.t)
        self.i += 1
        return self.t[k], self.b[k]


class Sched:
    ENGS = ("pe", "act", "dve", "pool", "sp")
    EMAP = {"pe": "tensor", "act": "scalar", "dve": "vector", "pool": "gpsimd", "sp": "sync"}

    def __init__(self, nc, es):
        self.nc = nc
        self.es = es
        self.prog = {e: [] for e in self.ENGS}
        self.sems = {}
        self.cnt = {}
        self.seen = {e: {} for e in self.ENGS}
        for e in self.ENGS:
            self._mksem("E_" + e)
        self.ndma = 0
        self.ninstr = 0

    def _mksem(self, key):
        self.sems[key] = self.es.enter_context(self.nc.semaphore("s_" + key))
        self.cnt[key] = 0
        return key

    def _need(self, reads, writes):
        need = {}
        for b in reads:
            if b.w is not None and need.get(b.w[0], 0) < b.w[1]:
                need[b.w[0]] = b.w[1]
        for b in writes:
            if b.w is not None and need.get(b.w[0], 0) < b.w[1]:
                need[b.w[0]] = b.w[1]
            for k, v in b.r.items():
                if need.get(k, 0) < v:
                    need[k] = v
        return need

    def _waits(self, eng, need):
        waits = []
        seen = self.seen[eng]
        for k, v in need.items():
            if seen.get(k, 0) < v:
                seen[k] = v
                waits.append((k, v))
        return waits

    def _force_inc(self, k, v, eng):
        e = k[2:]
        prog = self.prog[e]
        if v != self.cnt[k] + 1 or not prog or prog[-1][2] is not None or prog[-1][1] is None:
            raise RuntimeError("forward dep on %s from %s" % (k, eng))
        prog[-1][2] = (k, 1)
        self.cnt[k] += 1

    def op(self, eng, fn, reads=(), writes=(), inc=True):
        key = "E_" + eng
        need = self._need(reads, writes)
        if key in need and need[key] > self.cnt[key]:
            if eng != "pe":
                raise RuntimeError("same-engine forward dep on " + eng)
            del need[key]
        for k, v in need.items():
            if k.startswith("E_") and v > self.cnt[k]:
                self._force_inc(k, v, eng)
        waits = self._waits(eng, need)
        stamp = (key, self.cnt[key] + 1)
        if inc:
            self.cnt[key] += 1
        self.prog[eng].append([waits, fn, (key, 1) if inc else None])
        self.ninstr += 1
        for b in reads:
            if b.r.get(stamp[0], 0) < stamp[1]:
                b.r[stamp[0]] = stamp[1]
        for b in writes:
            b.w = stamp
            b.r = {}

    def dma(self, eng, out_ap, in_ap, reads=(), writes=(), serialize=True):
        sb = writes[0]
        if sb.dsem is None:
            sb.dsem = self._mksem("D%d_%s" % (self.ndma, sb.name))
            self.ndma += 1
        k = sb.dsem
        need = self._need(reads, writes)
        if not serialize and k in need:
            del need[k]
        for kk, v in need.items():
            if kk.startswith("E_") and v > self.cnt[kk]:
                self._force_inc(kk, v, eng)
        waits = self._waits(eng, need)
        self.cnt[k] += 16
        stamp = (k, self.cnt[k])

        def fn(e, out_ap=out_ap, in_ap=in_ap):
            return e.dma_start(out=out_ap, in_=in_ap)
        self.prog[eng].append([waits, fn, (k, 16)])
        self.ninstr += 1
        for b in reads:
            if b.r.get(k, 0) < stamp[1]:
                b.r[k] = stamp[1]
        for b in writes:
            b.w = stamp
            b.r = {}

    def barrier(self):
        for e in self.ENGS:
            waits = []
            for k, v in self.cnt.items():
                if v > 0 and k != "E_" + e and self.seen[e].get(k, 0) < v:
                    self.seen[e][k] = v
                    waits.append((k, v))
            own = "E_" + e
            if self.cnt[own] > 0 and self.seen[e].get(own, 0) < self.cnt[own]:
                self.seen[e][own] = self.cnt[own]
                waits.append((own, self.cnt[own]))
            self.prog[e].append([waits, None, None])

    def emit(self):
        with self.nc.Block() as block:
            for e in self.ENGS:
                prog = self.prog[e]
                if not prog:
                    continue

                def body(eng, prog=prog):
                    for waits, fn, inc in prog:
                        for k, v in waits:
                            eng.wait_ge(self.sems[k], v)
                        if fn is None:
                            continue
                        ins = fn(eng)
                        if inc is not None:
                            ins.then_inc(self.sems[inc[0]], inc[1])
                getattr(block, self.EMAP[e])(body)
        self.prog = {e: [] for e in self.ENGS}


class K:
    def __init__(self, S):
        self.S = S

    def act(self, out, in_, func, reads, writes, **kw):
        self.S.op("act", lambda e: e.activation(out=out, in_=in_, func=func, **kw), reads, writes)

    def mm(self, out, lhsT, rhs, start, stop, reads, writes, inc, skip=False):
        self.S.op("pe", lambda e: e.matmul(out, lhsT=lhsT, rhs=rhs, start=start, stop=stop,
                                           skip_group_check=skip), reads, writes, inc=inc)

    def tr(self, out, in_, ident, reads, writes, inc):
        self.S.op("pe", lambda e: e.transpose(out=out, in_=in_, identity=ident), reads, writes, inc=inc)

    def copy(self, eng, out, in_, reads, writes):
        if eng == "act":
            self.S.op("act", lambda e: e.activation(out=out, in_=in_, func=AF.Copy), reads, writes)
        else:
            self.S.op(eng, lambda e: e.tensor_copy(out=out, in_=in_), reads, writes)

    def tt(self, eng, out, in0, in1, op, reads, writes):
        self.S.op(eng, lambda e: e.tensor_tensor(out=out, in0=in0, in1=in1, op=op), reads, writes)

    def ts(self, eng, out, in0, s1, s2, op0, op1, reads, writes):
        if op1 is None:
            self.S.op(eng, lambda e: e.tensor_scalar(out=out, in0=in0, scalar1=s1, scalar2=None, op0=op0),
                      reads, writes)
        else:
            self.S.op(eng, lambda e: e.tensor_scalar(out=out, in0=in0, scalar1=s1, scalar2=s2, op0=op0, op1=op1),
                      reads, writes)

    def stt(self, out, in0, scalar, in1, op0, op1, reads, writes, accum_out=None):
        if accum_out is None:
            self.S.op("dve", lambda e: e.scalar_tensor_tensor(out=out, in0=in0, scalar=scalar, in1=in1,
                                                              op0=op0, op1=op1), reads, writes)
        else:
            self.S.op("dve", lambda e: e.scalar_tensor_tensor(out=out, in0=in0, scalar=scalar, in1=in1,
                                                              op0=op0, op1=op1, accum_out=accum_out), reads, writes)

    def memset(self, eng, ap, val, writes):
        self.S.op(eng, lambda e: e.memset(ap, val), (), writes)

    def asel(self, out, in_, pattern, op, fill, base, cm, reads, writes):
        self.S.op("pool", lambda e: e.affine_select(out=out, in_=in_, pattern=pattern, compare_op=op,
                                                    fill=fill, base=base, channel_multiplier=cm), reads, writes)

    def rmax(self, out, in_, reads, writes):
        self.S.op("dve", lambda e: e.reduce_max(out=out, in_=in_, axis=AX.X), reads, writes)

    def recip(self, out, in_, reads, writes):
        self.S.op("dve", lambda e: e.reciprocal(out=out, in_=in_), reads, writes)

    def scan(self, out, d0, d1, init, reads, writes):
        self.S.op("dve", lambda e: e.tensor_tensor_scan(out=out, data0=d0, data1=d1, initial=init,
                                                        op0=ALU.mult, op1=ALU.add), reads, writes)


def run_merged(main, side):
    n, m = len(main), len(side)
    done = 0
    for i, f in enumerate(main):
        f()
        want = (m * (i + 1)) // max(n, 1)
        while done < want:
            side[done]()
            done += 1
    while done < m:
        side[done]()
        done += 1


class Defer:
    def __init__(self, k, S, ops):
        self._k, self._S, self._ops = k, S, ops

    def dma(self, *a, **kw):
        self._ops.append(lambda: self._S.dma(*a, **kw))

    def __getattr__(self, name):
        f = getattr(self._k, name)

        def wrap(*a, **kw):
            self._ops.append(lambda: f(*a, **kw))
        return wrap


def build(stop_after=3, dbg=False):
    nc = bass.Bass("TRN2", target_bir_lowering=False)

    def din(name, shape, dt=F32):
        return nc.dram_tensor(name, shape, dt, kind="ExternalInput").ap()

    x = din("x", [S_LEN, D])
    mem = din("mem", [256, D])
    w_in = din("w_in", [D, 2560])
    w_out = din("w_out", [D, D])
    xa_q = din("xa_w_q", [D, D])
    xa_k = din("xa_w_k", [D, D])
    xa_v = din("xa_w_v", [D, D])
    xa_o = din("xa_w_o", [D, D])
    w_gate = din("w_gate", [16, D, 256])
    w_up = din("w_up", [16, D, 256])
    w_down = din("w_down", [16, 256, D])
    gains = din("gains", [5, 128, D])
    pp_d = din("pp", [128, 40])
    wbd_d = din("wbd", [128, 8, 128])
    wr_d = din("wr", [D, 20])
    rb_d = din("rb", [128, 20])
    y = nc.dram_tensor("y", [S_LEN, D], F32, kind="ExternalOutput").ap()
    skind = "ExternalOutput" if dbg else "Internal"
    x1s = nc.dram_tensor("x1s", [S_LEN, D], F32, kind=skind).ap()
    x2s = nc.dram_tensor("x2s", [S_LEN, D], F32, kind=skind).ap()
    dbg1 = nc.dram_tensor("dbg1", [128, 8, 512], BF16, kind=skind).ap()
    dbgB = Buf("dbg1")
    wg_s = nc.dram_tensor("wg_s", [16, 128, 8 * 256], BF16).ap()
    wu_s = nc.dram_tensor("wu_s", [16, 128, 8 * 256], BF16).ap()
    wd_s = nc.dram_tensor("wd_s", [16, 128, 2 * 1024], BF16).ap()

    _uid = [0]

    def _sbt(es_, n, s, d):
        _uid[0] += 1
        return es_.enter_context(nc.sbuf_tensor("%s_u%d" % (n, _uid[0]), s, d))

    def _pst(es_, n, s, d):
        _uid[0] += 1
        return es_.enter_context(nc.psum_tensor("%s_u%d" % (n, _uid[0]), s, d))

    with ExitStack() as eg:
        S = Sched(nc, eg)
        k = K(S)
        k_rec = k
        gsb = lambda n, s, d: _sbt(eg, n, s, d)
        ident = gsb("ident", [128, 128], BF16)
        negU = gsb("negU", [128, 128], BF16)
        negOnes = gsb("negOnes", [128, 128], BF16)
        ones = gsb("ones", [128, 128], BF16)
        maskneg = gsb("maskneg", [128, 128], BF16)
        maskfull = gsb("maskfull", [128, 512], BF16)
        sel = gsb("sel", [128, 16, 128], BF16)
        cst = gsb("cst", [128, 4], F32)
        pp = gsb("pp", [128, 40], F32)
        c12 = gsb("c12", [128, 20], F32)
        constB = Buf("const")
        ppB = Buf("pp")
        _wsB = Buf("wscr")
        wgsB = [_wsB for e in range(16)]
        wusB = [_wsB for e in range(16)]
        wdsB = [_wsB for e in range(16)]
        _x1B = [Buf("x1s%d" % i) for i in range(4)]
        _x2B = [Buf("x2s%d" % i) for i in range(4)]
        x1B = [_x1B[i % 4] for i in range(32)]
        x2B = [_x2B[i % 4] for i in range(32)]

        k.memset("pool", ident[:], 1.0, [constB])
        k.asel(ident[:], ident[:], [[-1, 128]], ALU.is_equal, 0.0, 0, 1, [constB], [constB])
        k.memset("pool", negU[:], -1.0, [constB])
        k.asel(negU[:], negU[:], [[-1, 128]], ALU.is_ge, 0.0, 0, 1, [constB], [constB])
        k.memset("pool", negOnes[:], -1.0, [constB])
        k.memset("pool", ones[:], 1.0, [constB])
        k.memset("pool", maskneg[:], NEG, [constB])
        k.asel(maskneg[:], maskneg[:], [[-1, 128]], ALU.is_ge, 0.0, 0, 1, [constB], [constB])
        k.memset("pool", maskfull[:], 0.0, [constB])
        k.copy("pool", maskfull[:, 0:128], maskneg[:], [constB], [constB])
        k.memset("pool", sel[:], 1.0, [constB])
        k.asel(sel[:], sel[:], [[-1, 16], [0, 128]], ALU.is_equal, 0.0, 0, 1, [constB], [constB])
        k.memset("pool", cst[:, 0:1], EPS, [constB])
        k.memset("pool", cst[:, 1:2], 1e-18, [constB])
        k.memset("pool", cst[:, 2:3], 1.0, [constB])
        S.dma("sp", pp[:], pp_d[:, :], writes=[ppB])
        k.act(c12[:, 0:4], pp[:, 28:32], AF.Exp, [ppB], [constB], scale=-1.0)
        k.act(c12[:, 0:4], c12[:, 0:4], AF.Ln, [constB], [constB], bias=cst[:, 2:3])
        k.ts("dve", c12[:, 4:8], c12[:, 0:4], -8.0, None, ALU.mult, None, [constB], [constB])
        k.ts("dve", c12[:, 8:12], c12[:, 0:4], -16.0, None, ALU.mult, None, [constB], [constB])
        moe_cvt = []
        for e in range(16):
            moe_cvt.append(lambda e=e: S.dma("pool", wg_s[e].rearrange("p (c f) -> p c f", c=8),
                                             w_gate[e].rearrange("(c p) f -> p c f", p=128), writes=[wgsB[e]],
                                             serialize=False))
            moe_cvt.append(lambda e=e: S.dma("pool", wu_s[e].rearrange("p (c f) -> p c f", c=8),
                                             w_up[e].rearrange("(c p) f -> p c f", p=128), writes=[wusB[e]],
                                             serialize=False))
            moe_cvt.append(lambda e=e: S.dma("pool", wd_s[e].rearrange("p (c f) -> p c f", c=2),
                                             w_down[e].rearrange("(c p) f -> p c f", p=128), writes=[wdsB[e]],
                                             serialize=False))

        G = dict(ident=ident, negU=negU, negOnes=negOnes, ones=ones, maskneg=maskneg, sel=sel, pp=pp,
                 c12=c12, constB=constB, ppB=ppB)

        def norm_T(es_tiles, xs, xsB, gain, gainB, dstT, dstB, j, evac_eng, kx=None, sq_dve=False):
            k = kx if kx is not None else k_rec
            ss, ssB = es_tiles["ss"].next()
            hn, hnB = es_tiles["hn"].next()
            pT, pTB = es_tiles["pT"].next()
            if sq_dve:
                k.stt(hn[:, 0:512], xs[:, 0:512], 1.0, xs[:, 0:512], ALU.mult, ALU.mult, [xsB], [hnB, ssB],
                      accum_out=ss[:, 0:1])
                k.stt(hn[:, 512:1024], xs[:, 512:1024], 1.0, xs[:, 512:1024], ALU.mult, ALU.mult, [xsB], [hnB, ssB],
                      accum_out=ss[:, 3:4])
                k.tt("dve", ss[:, 0:1], ss[:, 0:1], ss[:, 3:4], ALU.add, [ssB], [ssB])
            else:
                k.act(hn[:], xs, AF.Square, [xsB], [hnB, ssB], accum_out=ss[:, 0:1])
            k.act(ss[:, 1:2], ss[:, 0:1], AF.Ln, [ssB, constB], [ssB], scale=1.0 / D, bias=cst[:, 0:1])
            k.act(ss[:, 2:3], ss[:, 1:2], AF.Exp, [ssB], [ssB], scale=-0.5)
            for h_ in range(2):
                k.stt(hn[:, h_ * 512:(h_ + 1) * 512], xs[:, h_ * 512:(h_ + 1) * 512], ss[:, 2:3],
                      gain[:, h_ * 512:(h_ + 1) * 512], ALU.mult, ALU.mult, [xsB, ssB, gainB], [hnB])
            for dc in range(8):
                k.tr(pT[:, dc * 128:(dc + 1) * 128], hn[:, dc * 128:(dc + 1) * 128], ident[:],
                     [hnB, constB], [pTB], inc=(dc == 7))
            k.copy(evac_eng, dstT[:, :, j * 128:(j + 1) * 128], pT[:].rearrange("p (c t) -> p c t", c=8),
                   [pTB], [dstB])
            return ss, ssB

        def mk_norm_tiles(es, nc_):
            sb = lambda n, s, d: _sbt(es, n, s, d)
            sst = [sb("ss%d" % i, [128, 4], F32) for i in range(3)]
            t = dict(
                ss=Rot(sst, "ss"),
                hn=Rot([sb("hn%d" % i, [128, D], BF16) for i in range(2)], "hn"),
                pT=Rot([_pst(es, "pT%d" % i, [128, 1024], BF16) for i in range(1)], "pT"),
            )
            return t

        with ExitStack() as es:
            sb = lambda n, s, d: _sbt(es, n, s, d)
            psb = lambda n: _pst(es, n, [128, 512], F32)
            wbd = sb("wbd", [128, 8, 128], BF16)
            gmix = sb("gmix", [128, D], F32)
            KT = sb("KT", [128, 4, S_LEN], BF16)
            V = sb("V", [128, 32, 512], BF16)
            hal = sb("hal", [128, 4, 4], F32)
            hst = sb("hst", [128, 4], F32)
            gg = sb("gg", [128, 4, 512], BF16)
            hT = sb("hT", [128, 8, 512], BF16)
            qT2 = [(sb("qT%d" % i, [128, 8, 512], BF16), Buf("qT%d" % i)) for i in range(2)]
            yT2 = [(sb("yT%d" % i, [128, 8, 512], BF16), Buf("yT%d" % i)) for i in range(2)]
            rstdF = sb("rstdF", [128, 512], F32)
            rstdK = rstdF
            winR = Rot([sb("win%d" % i, [128, 8, 256], BF16) for i in range(4)], "win")
            woR = Rot([sb("wo%d" % i, [128, 8, 512], BF16) for i in range(2)], "wo")
            xsR = Rot([sb("xs%d" % i, [128, D], F32) for i in range(2)], "xs")
            xhR = Rot([sb("xh%d" % i, [128, 512], F32) for i in range(2)], "xh")
            t512 = lambda n, d, c: Rot([sb("%s%d" % (n, i), [128, 512], d) for i in range(c)], n)
            LT = []
            for i in range(2):
                T = {"xrt": sb("xrt%d" % i, [128, 515], F32), "xcb": sb("xcb%d" % i, [128, 512], BF16)}
                for n_ in ("xc", "r_", "ig", "b_"):
                    T[n_] = sb("%s%d" % (n_, i), [128, 512], F32)
                for n_ in ("xrtB", "xcB", "rB", "igB", "bB", "xcbB"):
                    T[n_] = Buf("%s%d" % (n_, i))
                LT.append(T)
            eR = Rot([sb("e_%d" % i, [128, 512], F32) for i in range(2)], "e")
            halBs = [Buf("hal%d" % i) for i in range(4)]
            hstBs = [Buf("hst%d" % i) for i in range(4)]
            sqR = t512("sq", BF16, 2)
            spR = t512("sp", BF16, 3)
            wR = t512("w", BF16, 3)
            RR = t512("R", BF16, 2)
            pmR = Rot([psb("pm%d" % i) for i in range(2)], "pm")
            ZR = Rot([psb("Z%d" % i) for i in range(4)], "Z")
            OR = Rot([psb("O%d" % i) for i in range(1)], "O")
            nt = mk_norm_tiles(es, nc)
            wbdB, gmixB = Buf("wbd"), Buf("gmix")
            KTb = [Buf("KT%d" % i) for i in range(NT)]
            Vb = [Buf("V%d" % i) for i in range(NT)]
            halB, hstB, ggB, hTB, rstdFB = (Buf("hal"), Buf("hst"), Buf("gg"), Buf("hT"), Buf("rstdF"))
            rstdKB = rstdFB
            S.dma("pool", wbd[:], wbd_d[:, :, :], writes=[wbdB])
            S.dma("sp", gmix[:], gains[0], writes=[gmixB])
            k.memset("pool", hal[:], 0.0, halBs)
            k.memset("pool", hst[:], 0.0, hstBs)
            for i in range(2):
                k.memset("pool", qT2[i][0][:], 0.0, [qT2[i][1]])
            k.ts("dve", c12[:, 12:20], pp[:, 20:28], -1.0, None, ALU.mult, None, [ppB], [constB])

            blocks = [4, 5, 6, 7, 8, 9, 2, 3, 0, 1]
            wseq = [(qt_, b) for qt_ in range(NT) for b in blocks]
            wst = {"n": 0, "no": 0}
            wtiles, wotiles = {}, {}

            def get_w(kk, s):
                while wst["n"] < min(s + 3, len(wseq)):
                    i = wst["n"]
                    b = wseq[i][1]
                    t, tB = winR.next()
                    kk.dma("pool", t[:], w_in[:, b * 256:(b + 1) * 256].rearrange("(c p) f -> p c f", p=128),
                          writes=[tB])
                    wtiles[i] = (t, tB)
                    wst["n"] += 1
                return wtiles[s]

            def get_wo(kk, s):
                while wst["no"] < min(s + 2, 2 * NT):
                    i = wst["no"]
                    half = i % 2
                    t, tB = woR.next()
                    kk.dma("pool", t[:], w_out[:, half * 512:(half + 1) * 512].rearrange("(c p) f -> p c f", p=128),
                          writes=[tB])
                    wotiles[i] = (t, tB)
                    wst["no"] += 1
                return wotiles[s]

            def proj(kk, wt, wtB, lo):
                pa, paB = pmR.next()
                for dc in range(8):
                    kk.mm(pa[:, :], wt[:, dc, lo:lo + 128], hT[:, dc, :], dc == 0, dc == 7,
                         [wtB, hTB], [paB], inc=(dc == 7))
                return pa, paB

            def sigmoid_from(kk, dst, dstB, pa, paB, nbias, on_act=False):
                kk.act(dst[:], pa[:, :], AF.Exp, [paB, constB], [dstB], scale=-1.0, bias=nbias)
                if on_act:
                    kk.act(dst[:], dst[:], AF.Ln, [dstB, constB], [dstB], bias=cst[:, 2:3])
                    kk.act(dst[:], dst[:], AF.Exp, [dstB], [dstB], scale=-1.0)
                else:
                    kk.ts("dve", dst[:], dst[:], 1.0, None, ALU.add, None, [dstB], [dstB])
                    for q_ in range(4):
                        kk.recip(dst[:, q_ * 128:(q_ + 1) * 128], dst[:, q_ * 128:(q_ + 1) * 128], [dstB], [dstB])

            def lru_chain(kk, qt, c, wt, wtB, lo, T, bank, yTt, yTB):
                pa, paB = bank
                for dc in range(8):
                    kk.mm(pa[:, :], wt[:, dc, lo:lo + 128], hT[:, dc, :], dc == 0, dc == 7,
                          [wtB, hTB], [paB], inc=(dc == 7))
                kk.copy("dve", T["xrt"][:, 0:3], hal[:, c, 0:3], [halBs[c]], [T["xrtB"]])
                kk.copy("dve", T["xrt"][:, 3:515], pa[:, :], [paB], [T["xrtB"]])
                kk.ts("dve", T["xc"][:], T["xrt"][:, 0:512], pp[:, c * 4:c * 4 + 1], pp[:, 16 + c:17 + c],
                     ALU.mult, ALU.add, [T["xrtB"], ppB], [T["xcB"]])
                for t in range(1, 4):
                    kk.stt(T["xc"][:], T["xrt"][:, t:t + 512], pp[:, c * 4 + t:c * 4 + t + 1], T["xc"][:],
                          ALU.mult, ALU.add, [T["xrtB"], ppB, T["xcB"]], [T["xcB"]])
                kk.copy("dve", hal[:, c, 0:3], T["xrt"][:, 512:515], [T["xrtB"]], [halBs[c]])
                kk.copy("dve", T["xcb"][:], T["xc"][:], [T["xcB"]], [T["xcbB"]])
                pg, pgB = bank
                kk.mm(pg[:, :], wbd[:, c, :], T["xcb"][:], True, True, [wbdB, T["xcbB"]], [pgB], inc=True)
                sigmoid_from(kk, T["r_"], T["rB"], pg, pgB, c12[:, 12 + c:13 + c], on_act=(qt <= 3))
                pg2, pg2B = bank
                kk.mm(pg2[:, :], wbd[:, 4 + c, :], T["xcb"][:], True, True, [wbdB, T["xcbB"]], [pg2B], inc=True)
                sigmoid_from(kk, T["ig"], T["igB"], pg2, pg2B, c12[:, 16 + c:17 + c], on_act=(qt <= 3))
                kk.act(T["b_"][:], T["r_"][:], AF.Exp, [T["rB"], constB], [T["bB"]], scale=c12[:, 8 + c:9 + c])
                kk.act(T["r_"][:], T["r_"][:], AF.Exp, [T["rB"], constB], [T["rB"]], scale=c12[:, 4 + c:5 + c])
                kk.ts("dve", T["b_"][:], T["b_"][:], 0.99999994, -1.0, ALU.min, ALU.mult, [T["bB"]], [T["bB"]])
                kk.act(T["b_"][:], T["b_"][:], AF.Ln, [T["bB"], constB], [T["bB"]], bias=cst[:, 2:3])
                kk.act(T["b_"][:], T["b_"][:], AF.Exp, [T["bB"]], [T["bB"]], scale=0.5)
                kk.tt("dve", T["ig"][:], T["ig"][:], T["xc"][:], ALU.mult, [T["igB"], T["xcB"]], [T["igB"]])
                kk.tt("dve", T["b_"][:], T["b_"][:], T["ig"][:], ALU.mult, [T["bB"], T["igB"]], [T["bB"]])
                kk.scan(T["xc"][:, 0:256], T["r_"][:, 0:256], T["b_"][:, 0:256], hst[:, c:c + 1],
                        [T["rB"], T["bB"], hstBs[c], T["xcB"]], [T["xcB"]])
                kk.scan(T["xc"][:, 256:512], T["r_"][:, 256:512], T["b_"][:, 256:512], T["xc"][:, 255:256],
                        [T["rB"], T["bB"], T["xcB"]], [T["xcB"]])
                kk.copy("dve", hst[:, c:c + 1], T["xc"][:, 511:512], [T["xcB"]], [hstBs[c]])
                kk.tt("dve", yTt[:, c, :], T["xc"][:], gg[:, c, :], ALU.mult, [T["xcB"], ggB], [yTB])
                sq, sqB = sqR.next()
                kk.tt("dve", sq[:], yTt[:, c, :], yTt[:, c, :], ALU.mult, [yTB], [sqB])
                ps_, psB_ = bank
                kk.mm(ps_[:, :], ones[:], sq[:], True, True, [constB, sqB], [psB_], inc=True)
                if c == 0:
                    kk.copy("dve", rstdF[:], ps_[:, :], [psB_], [rstdFB])
                else:
                    kk.tt("dve", rstdF[:], ps_[:, :], rstdF[:], ALU.add, [psB_, rstdFB], [rstdFB])

            fsplit = {}

            def front_ops(qt):
                ops = []
                kk = Defer(k, S, ops)
                cur = qt % 2
                qTt, qTB = qT2[cur]
                yTt, yTB = yT2[cur]
                for j in range(4):
                    n = qt * 4 + j
                    xs, xsB = xsR.next()
                    kk.dma("sp", xs[:], x[n * 128:(n + 1) * 128, :], writes=[xsB])
                    norm_T(nt, xs[:], xsB, gmix[:], gmixB, hT, hTB, j, "dve", kx=kk, sq_dve=(qt >= 5))
                ssP = None
                for bi, b in enumerate(blocks):
                    if b == 2:
                        fsplit[qt] = len(ops)
                    wt, wtB = get_w(kk, qt * 10 + bi)
                    if b in (8, 9):
                        for j in range(4):
                            pa, paB = pmR.next()
                            for dc in range(8):
                                kk.mm(pa[:, 0:256], hT[:, dc, j * 128:(j + 1) * 128], wt[:, dc, :], dc == 0, dc == 7,
                                     [wtB, hTB], [paB], inc=(dc == 7))
                            kk.copy("dve", V[:, qt * 4 + j, (b - 8) * 256:(b - 7) * 256], pa[:, 0:256], [paB], [Vb[qt]])
                        continue
                    if b in (0, 1):
                        chains = []
                        for half in range(2):
                            cops = []
                            lru_chain(Defer(k, S, cops), qt, b * 2 + half, wt, wtB, half * 128, LT[half],
                                      (pmR.t[half], pmR.b[half]), yTt, yTB)
                            chains.append(cops)
                        for i in range(max(len(chains[0]), len(chains[1]))):
                            for cops in chains:
                                if i < len(cops):
                                    ops.append(cops[i])
                        continue
                    for half in range(2):
                        pa, paB = proj(kk, wt, wtB, half * 128)
                        if b in (4, 5):
                            c = (b - 4) * 2 + half
                            kk.ts("dve", qTt[0:64, 2 * c, :], pa[0:64, :], 0.125, None, ALU.mult, None, [paB], [qTB])
                            kk.ts("dve", qTt[64:128, 2 * c + 1, :], pa[64:128, :], 0.125, None, ALU.mult, None,
                                 [paB], [qTB])
                        elif b in (6, 7):
                            c = (b - 6) * 2 + half
                            kk.copy("dve", KT[:, c, qt * 512:(qt + 1) * 512], pa[:, :], [paB], [KTb[qt]])
                        elif b in (2, 3):
                            c = (b - 2) * 2 + half
                            kk.copy("dve", gg[:, c, :], pa[:, :], [paB], [ggB])
                            if c == 3:
                                def _gelu4():
                                    for c_ in range(4):
                                        k_rec.act(gg[:, c_, :], gg[:, c_, :], AF.Gelu_apprx_tanh, [ggB], [ggB])
                                ops.append(_gelu4)
                kk.act(rstdF[:], rstdF[:], AF.Ln, [rstdFB, constB], [rstdFB], scale=1.0 / 512, bias=cst[:, 0:1])
                kk.act(rstdF[:], rstdF[:], AF.Exp, [rstdFB], [rstdFB], scale=-0.5)
                for c in range(4):
                    kk.stt(yTt[:, c, :], yTt[:, c, :], pp[:, 32 + c:33 + c], rstdF[:], ALU.mult, ALU.mult,
                          [yTB, ppB, rstdFB], [yTB])
                return ops

            def back_ops(qt):
                ops = []
                kk = Defer(k, S, ops)
                yTt, yTB = yT2[qt % 2]
                ssP, ssPB = pmR.next()
                for pr in range(4):
                    sq, sqB = sqR.next()
                    kk.tt("dve", sq[:], yTt[:, 4 + pr, :], yTt[:, 4 + pr, :], ALU.mult, [yTB], [sqB])
                    kk.mm(ssP[:, :], ones[:], sq[:], pr == 0, pr == 3, [constB, sqB], [ssPB], inc=True)
                kk.act(rstdK[:], ssP[:, :], AF.Ln, [ssPB, constB], [rstdKB], scale=1.0 / 512, bias=cst[:, 0:1])
                kk.act(rstdK[:], rstdK[:], AF.Exp, [rstdKB], [rstdKB], scale=-0.5)
                for pr in range(4):
                    kk.stt(yTt[:, 4 + pr, :], yTt[:, 4 + pr, :], pp[:, 36 + pr:37 + pr], rstdK[:], ALU.mult, ALU.mult,
                          [yTB, ppB, rstdKB], [yTB])
                if dbg and qt == 0:
                    kk.dma("sp", dbg1[:, :, :], yTt[:], reads=[yTB], writes=[dbgB])
                for half in range(2):
                    wo_, woB_ = get_wo(kk, qt * 2 + half)
                    for j in range(4):
                        n = qt * 4 + j
                        xh, xhB = xhR.next()
                        kk.dma("sp", xh[:], x[n * 128:(n + 1) * 128, half * 512:(half + 1) * 512], writes=[xhB])
                        pa, paB = pmR.next()
                        for c in range(8):
                            kk.mm(pa[:, :], yTt[:, c, j * 128:(j + 1) * 128], wo_[:, c, :], c == 0, c == 7,
                                 [yTB, woB_], [paB], inc=(c == 7))
                        kk.tt("dve", xh[:], pa[:, :], xh[:], ALU.add, [paB, xhB], [xhB])
                        kk.dma("sp", x1s[n * 128:(n + 1) * 128, half * 512:(half + 1) * 512], xh[:], reads=[xhB],
                              writes=[x1B[n]])
                return ops

            def attention(qt, side):
                qTt, qTB = qT2[qt % 2]
                yTt, yTB = yT2[qt % 2]
                items = []
                for h in range(8):
                    kmax = 4 * qt + 3
                    for kb in range(kmax, -1, -1):
                        jd = kb - 4 * qt
                        c0 = jd * 128 if jd > 0 else 0
                        items.append(dict(h=h, kb=kb, c0=c0, diag=(jd >= 0), first=(kb == kmax), last=(kb == 0)))
                n_it = len(items)
                st = {}

                def s0(i):
                    it = items[i]
                    h, kb, c0 = it["h"], it["kb"], it["c0"]
                    Z, ZB = ZR.next()
                    it["Z"], it["ZB"] = Z, ZB
                    k.mm(Z[:, c0:512], KT[:, h // 2, kb * 128:(kb + 1) * 128], qTt[:, h, c0:512],
                         True, not it["diag"], [KTb[kb // 4], qTB], [ZB], inc=(not it["diag"]))
                    if it["diag"]:
                        k.mm(Z[:, c0:512], ident[:], maskfull[:, 0:512 - c0], False, True, [constB], [ZB], inc=True)
                    if it["first"]:
                        R, RB = RR.next()
                        st[("R", h)] = (R, RB)
                        k.memset("dve", R[:], 0.0, [RB])

                def s1a(i):
                    it = items[i]
                    c0 = it["c0"]
                    e_, eB = eR.next()
                    it["e"], it["eB"] = e_, eB
                    k.act(e_[:, c0:512], it["Z"][:, c0:512], AF.Exp, [it["ZB"]], [eB])

                def s1b(i):
                    it = items[i]
                    c0 = it["c0"]
                    sp, spB = spR.next()
                    it["sp"], it["spB"] = sp, spB
                    k.act(sp[:, c0:512], it["e"][:, c0:512], AF.Ln, [it["eB"], constB], [spB], bias=cst[:, 2:3])

                def s3(i):
                    it = items[i]
                    c0 = it["c0"]
                    R, RB = st[("R", it["h"])]
                    Z, ZB = it["Z"], it["ZB"]
                    k.mm(Z[:, c0:512], negU[:], it["sp"][:, c0:512], False, it["first"], [constB, it["spB"]], [ZB],
                         inc=it["first"], skip=True)
                    if not it["first"]:
                        k.mm(Z[:, c0:512], negOnes[:], R[:, c0:512], False, True, [constB, RB], [ZB], inc=True,
                             skip=True)
                    if not it["last"]:
                        k.tt("dve", R[:, c0:512], R[:, c0:512], it["sp"][:, c0:512], ALU.add, [RB, it["spB"]], [RB])

                def s4(i):
                    it = items[i]
                    c0 = it["c0"]
                    w, wB = wR.next()
                    it["w"], it["wB"] = w, wB
                    k.act(w[:, c0:512], it["Z"][:, c0:512], AF.Exp, [it["ZB"]], [wB])

                def s5(i):
                    it = items[i]
                    h, kb, c0 = it["h"], it["kb"], it["c0"]
                    pr, hp = h // 2, (h % 2) * 64
                    if it["first"]:
                        st["O"] = OR.next()
                    O, OB = st["O"]
                    k.mm(O[:, c0:512], V[:, kb, pr * 128:(pr + 1) * 128], it["w"][:, c0:512],
                         it["first"], it["last"], [Vb[kb // 4], it["wB"]], [OB], inc=it["last"], skip=True)
                    if it["last"]:
                        k.copy("dve", yTt[hp:hp + 64, 4 + pr, :], O[hp:hp + 64, :], [OB], [yTB])

                steps = n_it + 4
                n_side = len(side)
                done = 0
                for ti, t in enumerate(range(-3, n_it + 1)):
                    if 0 <= t + 3 < n_it:
                        s0(t + 3)
                    if 0 <= t + 1 < n_it:
                        s1b(t + 1)
                    if 0 <= t + 2 < n_it:
                        s1a(t + 2)
                    if 0 <= t < n_it:
                        s3(t)
                        s4(t)
                    if 0 <= t - 1 < n_it:
                        s5(t - 1)
                    want = (n_side * (ti + 1)) // steps
                    while done < want:
                        side[done]()
                        done += 1
                while done < n_side:
                    side[done]()
                    done += 1

            f0 = front_ops(0)
            for f in f0[:fsplit[0]]:
                f()
            for qt in range(NT):
                side = []
                if qt == 0:
                    side += f0[fsplit[0]:]
                if qt >= 1:
                    side += back_ops(qt - 1)
                if qt + 1 < NT:
                    side += front_ops(qt + 1)
                side += moe_cvt[qt * 6:(qt + 1) * 6]
                attention(qt, side)
            for f in back_ops(NT - 1):
                f()
            S.barrier()
            S.emit()
        if stop_after == 1:
            return nc

        with ExitStack() as es:
            sb = lambda n, s, d: _sbt(es, n, s, d)
            psb = lambda n: _pst(es, n, [128, 512], F32)
            wq = sb("wq", [128, 8, D], BF16)
            wo = sb("wo", [128, 8, D], BF16)
            gxa = sb("gxa", [128, D], F32)
            kmT = sb("kmT", [128, 8, 256], BF16)
            vm = sb("vm", [128, 2, D], BF16)
            qT2R = Rot([sb("qT2_%d" % i, [128, 8, 512], BF16) for i in range(2)], "qT2")
            pTsR = Rot([sb("pT_sb%d" % i, [128, 8, 512], BF16) for i in range(2)], "pTs")
            oTR = Rot([sb("oT%d" % i, [128, 8, 512], BF16) for i in range(2)], "oT")
            hT = Rot([sb("h2T%d" % i, [128, 8, 512], BF16) for i in range(2)], "h2T")
            xrR = Rot([sb("xr%d" % i, [128, D], F32) for i in range(2)], "xr")
            xsR = Rot([sb("xs%d" % i, [128, D], F32) for i in range(2)], "xs")
            pexR = Rot([sb("pex%d" % i, [128, 4, 256], F32) for i in range(2)], "pex")
            pnR = Rot([sb("pn%d" % i, [128, 4, 256], BF16) for i in range(2)], "pn")
            smR = Rot([sb("sm%d" % i, [128, 16], F32) for i in range(2)], "sm")
            pmR = Rot([psb("pm%d" % i) for i in range(2)], "pm")
            pqR = Rot([psb("pq%d" % i) for i in range(2)], "pq")
            scR = Rot([psb("sc%d" % i) for i in range(2)], "sc")
            ptR = Rot([_pst(es, "pt2_%d" % i, [128, 1024], BF16) for i in range(1)], "pt2")
            nt = mk_norm_tiles(es, nc)
            wqB, woB, gxaB, kmTB, vmB = (Buf("wq"), Buf("wo"), Buf("gxa"), Buf("kmT"), Buf("vm"))
            S.dma("pool", wq[:], xa_q.rearrange("(c p) f -> p c f", p=128), writes=[wqB])
            S.dma("pool", wo[:], xa_o.rearrange("(c p) f -> p c f", p=128), writes=[woB])
            S.dma("sp", gxa[:], gains[1], writes=[gxaB])
            with ExitStack() as es0:
                sb0 = lambda n, s, d: _sbt(es0, n, s, d)
                wk = sb0("wk", [128, 8, D], BF16)
                wv = sb0("wv", [128, 8, D], BF16)
                gm = sb0("gm", [128, D], F32)
                mT = sb0("mT", [128, 8, 512], BF16)
                wkB, wvB, gmB, mTB = Buf("wk"), Buf("wv"), Buf("gm"), Buf("mT")
                S.dma("pool", wk[:], xa_k.rearrange("(c p) f -> p c f", p=128), writes=[wkB])
                S.dma("pool", wv[:], xa_v.rearrange("(c p) f -> p c f", p=128), writes=[wvB])
                S.dma("sp", gm[:], gains[4], writes=[gmB])
                for j in range(2):
                    xs, xsB = xsR.next()
                    S.dma("sp", xs[:], mem[j * 128:(j + 1) * 128, :], writes=[xsB])
                    norm_T(nt, xs[:], xsB, gm[:], gmB, mT, mTB, j, "dve")
                for fc in range(8):
                    pa, paB = pmR.next()
                    for dc in range(8):
                        k.mm(pa[:, 0:256], wk[:, dc, fc * 128:(fc + 1) * 128], mT[:, dc, 0:256], dc == 0, dc == 7,
                             [wkB, mTB], [paB], inc=(dc == 7))
                    k.copy("act", kmT[:, fc, :], pa[:, 0:256], [paB], [kmTB])
                for mc in range(2):
                    for half in range(2):
                        pa, paB = pmR.next()
                        for dc in range(8):
                            k.mm(pa[:, :], mT[:, dc, mc * 128:(mc + 1) * 128], wv[:, dc, half * 512:(half + 1) * 512],
                                 dc == 0, dc == 7, [wvB, mTB], [paB], inc=(dc == 7))
                        k.copy("dve", vm[:, mc, half * 512:(half + 1) * 512], pa[:, :], [paB], [vmB])
                S.barrier()
                S.emit()
            st2 = {}

            def F2(qt):
                ops = []
                kk = Defer(k, S, ops)
                hTt, hTB = hT.next()
                qT2, qT2B = qT2R.next()
                st2[qt] = (qT2, qT2B)
                for j in range(4):
                    n = qt * 4 + j
                    xs, xsB = xsR.next()
                    kk.dma("sp", xs[:], x1s[n * 128:(n + 1) * 128, :], reads=[x1B[n]], writes=[xsB])
                    norm_T(nt, xs[:], xsB, gxa[:], gxaB, hTt, hTB, j, "dve", kx=kk)
                for fc in range(8):
                    pa, paB = pqR.next()
                    for dc in range(8):
                        kk.mm(pa[:, :], wq[:, dc, fc * 128:(fc + 1) * 128], hTt[:, dc, :], dc == 0, dc == 7,
                             [wqB, hTB], [paB], inc=(dc == 7))
                    if fc % 2 == 0:
                        kk.act(qT2[:, fc, :], pa[:, :], AF.Copy, [paB], [qT2B], scale=0.0625)
                    else:
                        kk.ts("dve", qT2[:, fc, :], pa[:, :], 0.0625, None, ALU.mult, None, [paB], [qT2B])
                return ops

            def MB2(qt):
                ops = []
                kk = Defer(k, S, ops)
                qT2, qT2B = st2[qt]
                pT_sb, pTsB = pTsR.next()
                oT, oTB = oTR.next()
                for j in range(4):
                    pex, pexB = pexR.next()
                    pn, pnB = pnR.next()
                    sm, smB = smR.next()
                    for hp2 in range(2):
                        sc, scB = scR.next()
                        for hh in (2 * hp2, 2 * hp2 + 1):
                            for k2 in range(2):
                                fc = hh * 2 + k2
                                kk.mm(sc[:, (hh % 2) * 256:(hh % 2 + 1) * 256], qT2[:, fc, j * 128:(j + 1) * 128],
                                     kmT[:, fc, :], k2 == 0, k2 == 1, [qT2B, kmTB], [scB],
                                     inc=(k2 == 1 and hh % 2 == 1), skip=True)
                        kk.rmax(sm[:, 2 * hp2:2 * hp2 + 2], sc[:].rearrange("p (h m) -> p h m", h=2), [scB], [smB])
                        kk.ts("dve", sm[:, 4 + 2 * hp2:6 + 2 * hp2], sm[:, 2 * hp2:2 * hp2 + 2], -1.0, None, ALU.mult,
                             None, [smB], [smB])
                        for hh in (2 * hp2, 2 * hp2 + 1):
                            kk.act(pex[:, hh, :], sc[:, (hh % 2) * 256:(hh % 2 + 1) * 256], AF.Exp, [scB, smB],
                                  [pexB, smB], bias=sm[:, 4 + hh:5 + hh], accum_out=sm[:, 8 + hh:9 + hh])
                    kk.recip(sm[:, 12:16], sm[:, 8:12], [smB], [smB])
                    for hh in range(4):
                        kk.ts("dve", pn[:, hh, :], pex[:, hh, :], sm[:, 12 + hh:13 + hh], None, ALU.mult, None,
                             [pexB, smB], [pnB])
                    pt, ptB = ptR.next()
                    for hh in range(4):
                        for mc in range(2):
                            q8 = hh * 2 + mc
                            kk.tr(pt[:, q8 * 128:(q8 + 1) * 128], pn[:, hh, mc * 128:(mc + 1) * 128], ident[:],
                                 [pnB, constB], [ptB], inc=(q8 == 7))
                    kk.copy("act", pT_sb[:, :, j * 128:(j + 1) * 128], pt[:].rearrange("p (c t) -> p c t", c=8),
                           [ptB], [pTsB])
                for fc in range(8):
                    hh = fc // 2
                    pa, paB = pmR.next()
                    for mc in range(2):
                        kk.mm(pa[:, :], vm[:, mc, fc * 128:(fc + 1) * 128], pT_sb[:, hh * 2 + mc, :], mc == 0, mc == 1,
                             [vmB, pTsB], [paB], inc=(mc == 1))
                    kk.copy("act" if fc % 2 == 0 else "dve", oT[:, fc, :], pa[:, :], [paB], [oTB])
                for j in range(4):
                    n = qt * 4 + j
                    xs, xsB = xrR.next()
                    kk.dma("sp", xs[:], x1s[n * 128:(n + 1) * 128, :], reads=[x1B[n]], writes=[xsB])
                    for half in range(2):
                        pa, paB = pmR.next()
                        for c in range(8):
                            kk.mm(pa[:, :], oT[:, c, j * 128:(j + 1) * 128], wo[:, c, half * 512:(half + 1) * 512],
                                 c == 0, c == 7, [oTB, woB], [paB], inc=(c == 7))
                        kk.tt("dve", xs[:, half * 512:(half + 1) * 512], pa[:, :], xs[:, half * 512:(half + 1) * 512],
                             ALU.add, [paB, xsB], [xsB])
                    kk.dma("sp", x2s[n * 128:(n + 1) * 128, :], xs[:], reads=[xsB], writes=[x2B[n]])
                return ops

            for f in F2(0):
                f()
            for qt in range(NT):
                run_merged(MB2(qt), F2(qt + 1) if qt + 1 < NT else [])
            S.barrier()
            S.emit()
        if stop_after == 2:
            return nc

        with ExitStack() as es:
            sb = lambda n, s, d: _sbt(es, n, s, d)
            psb = lambda n: _pst(es, n, [128, 512], F32)
            gmoe = sb("gmoe", [128, D], F32)
            gfin = sb("gfin", [128, D], F32)
            wr = sb("wr", [128, 8, 20], BF16)
            rb = sb("rb", [128, 20], F32)
            tT = Rot([sb("tT%d" % i, [128, 8, 512], BF16) for i in range(2)], "tT")
            accR = Rot([sb("acc%d" % i, [128, 4, D], F32) for i in range(2)], "acc")
            hidR = Rot([sb("hid%d" % i, [128, 16, 512], BF16) for i in range(2)], "hid")
            wgR = Rot([sb("wg%d" % i, [128, 8, 256], BF16) for i in range(4)], "wg")
            wuR = Rot([sb("wu%d" % i, [128, 8, 256], BF16) for i in range(4)], "wu")
            wdR = Rot([sb("wd%d" % i, [128, 2, D], BF16) for i in range(10)], "wd")
            combT = Rot([sb("combT%d" % i, [128, 512], BF16) for i in range(2)], "combT")
            cbR = Rot([sb("cb%d" % i, [128, 512], BF16) for i in range(2)], "cb")
            sgR = Rot([sb("sg%d" % i, [128, 512], F32) for i in range(4)], "sg")
            xsR = Rot([sb("xs%d" % i, [128, D], F32) for i in range(2)], "xs")
            yoR = Rot([sb("yo%d" % i, [128, D], F32) for i in range(2)], "yo")
            rtR = Rot([sb("rt%d" % i, [128, 96], F32) for i in range(2)], "rt")
            cmbR = Rot([sb("cmb%d" % i, [128, 128], BF16) for i in range(2)], "cmb")
            for i in range(2):
                k.memset("pool", cmbR.t[i][:], 0.0, [cmbR.b[i]])
            gR = Rot([psb("g%d" % i) for i in range(2)], "g")
            uR = Rot([psb("u%d" % i) for i in range(2)], "u")
            dnR = Rot([psb("dn%d" % i) for i in range(2)], "dn")
            rtP = Rot([psb("rtp%d" % i) for i in range(1)], "rtp")
            nt = mk_norm_tiles(es, nc)
            gmoeB, gfinB, wrB, rbB = Buf("gmoe"), Buf("gfin"), Buf("wr"), Buf("rb")
            yB = [Buf("y%d" % i) for i in range(4)]
            S.dma("sp", gmoe[:], gains[2], writes=[gmoeB])
            S.dma("sp", gfin[:], gains[3], writes=[gfinB])
            S.dma("pool", wr[:], wr_d.rearrange("(c p) f -> p c f", p=128), writes=[wrB])
            S.dma("sp", rb[:], rb_d[:, :], writes=[rbB])
            st3 = {}

            def front3(qt):
                ops = []
                kk = Defer(k, S, ops)
                tTt, tTB = tT.next()
                cT, cTB = combT.next()
                acc, accB = accR.next()
                st3[qt] = (tTt, tTB, cT, cTB, acc, accB)
                for j in range(4):
                    n = qt * 4 + j
                    xs, xsB = xsR.next()
                    kk.dma("sp", xs[:], x2s[n * 128:(n + 1) * 128, :], reads=[x2B[n]], writes=[xsB])
                    norm_T(nt, xs[:], xsB, gmoe[:], gmoeB, tTt, tTB, j, "dve", kx=kk)
                    for h_ in range(2):
                        kk.copy("dve", acc[:, j, h_ * 512:(h_ + 1) * 512], xs[:, h_ * 512:(h_ + 1) * 512], [xsB], [accB])
                    pm, pmB = rtP.next()
                    for dc in range(8):
                        kk.mm(pm[:, 0:20], tTt[:, dc, j * 128:(j + 1) * 128], wr[:, dc, :], dc == 0, dc == 7,
                             [tTB, wrB], [pmB], inc=(dc == 7))
                    rt, rtB = rtR.next()
                    cmb, cmbB = cmbR.next()
                    RW = dict(reads=[rtB], writes=[rtB])
                    lg = rt[:, 0:20]
                    kk.tt("dve", lg, pm[:, 0:20], rb[:, :], ALU.add, [pmB, rbB], [rtB])
                    gmax, ngmax, gmask, gexp, gsum, gp = (rt[:, 20:21], rt[:, 21:22], rt[:, 24:28], rt[:, 28:32],
                                                          rt[:, 22:23], rt[:, 23:24])
                    kk.rmax(gmax, rt[:, 0:4], **RW)
                    kk.ts("dve", ngmax, gmax, -1.0, None, ALU.mult, None, **RW)
                    kk.ts("dve", gmask, rt[:, 0:4], gmax, None, ALU.is_equal, None, **RW)
                    kk.act(gexp, rt[:, 0:4], AF.Exp, [rtB], [rtB], bias=ngmax, accum_out=gsum)
                    kk.recip(gp, gsum, **RW)
                    ch = rt[:, 32:36]
                    kk.ts("dve", ch, rt[:, 4:8], rt[:, 24:25], None, ALU.mult, None, **RW)
                    for g in range(1, 4):
                        kk.stt(ch, rt[:, 4 + 4 * g:8 + 4 * g], rt[:, 24 + g:25 + g], ch, ALU.mult, ALU.add, **RW)
                    m1, m2, mask1, c2, mask2 = rt[:, 36:37], rt[:, 37:38], rt[:, 40:44], rt[:, 44:48], rt[:, 48:52]
                    kk.rmax(m1, ch, **RW)
                    kk.ts("dve", mask1, ch, m1, None, ALU.is_equal, None, **RW)
                    kk.stt(c2, mask1, -1e30, ch, ALU.mult, ALU.add, **RW)
                    kk.rmax(m2, c2, **RW)
                    kk.ts("dve", mask2, c2, m2, None, ALU.is_equal, None, **RW)
                    dd, ed, w1, w2 = rt[:, 38:39], rt[:, 39:40], rt[:, 52:53], rt[:, 53:54]
                    kk.tt("dve", dd, m2, m1, ALU.subtract, **RW)
                    kk.act(ed, dd, AF.Exp, [rtB], [rtB])
                    kk.ts("dve", ed, ed, 1.0, None, ALU.add, None, **RW)
                    kk.recip(w1, ed, **RW)
                    kk.tt("dve", w1, w1, gp, ALU.mult, **RW)
                    kk.tt("dve", w2, gp, w1, ALU.subtract, **RW)
                    ew = rt[:, 56:60]
                    kk.ts("dve", ew, mask1, w1, None, ALU.mult, None, **RW)
                    kk.stt(ew, mask2, w2, ew, ALU.mult, ALU.add, **RW)
                    for g in range(4):
                        kk.ts("dve", cmb[:, 4 * g:4 * g + 4], ew, rt[:, 24 + g:25 + g], None, ALU.mult, None,
                             [rtB], [cmbB])
                    pt, ptB = nt["pT"].next()
                    kk.tr(pt[:, 0:128], cmb[:, :], ident[:], [cmbB, constB], [ptB], inc=True)
                    kk.copy("dve", cT[:, j * 128:(j + 1) * 128], pt[:, 0:128], [ptB], [cTB])
                return ops

            def experts3(qt, grp):
                ops = []
                kk = Defer(k, S, ops)
                tTt, tTB, cT, cTB, acc, accB = st3[qt]
                if True:
                    hid, hidB = hidR.next()
                    wds = []
                    for e8 in range(8):
                        E = grp * 8 + e8
                        wg, wgB = wgR.next()
                        wu, wuB = wuR.next()
                        wd, wdB = wdR.next()
                        wds.append((wd, wdB))
                        kk.dma("sp", wg[:], wg_s[E].rearrange("p (c f) -> p c f", c=8), reads=[wgsB[E]], writes=[wgB])
                        kk.dma("sp", wu[:], wu_s[E].rearrange("p (c f) -> p c f", c=8), reads=[wusB[E]], writes=[wuB])
                        kk.dma("sp", wd[:], wd_s[E].rearrange("p (c f) -> p c f", c=2), reads=[wdsB[E]], writes=[wdB])
                        cbp, cbpB = gR.next()
                        kk.mm(cbp[:, :], sel[:, E, :], cT[:, :], True, True, [constB, cTB], [cbpB], inc=True)
                        cb, cbB = cbR.next()
                        kk.copy("dve", cb[:], cbp[:, :], [cbpB], [cbB])
                        for fc in range(2):
                            gp_, gpB = gR.next()
                            up_, upB = uR.next()
                            for dc in range(8):
                                kk.mm(gp_[:, :], wg[:, dc, fc * 128:(fc + 1) * 128], tTt[:, dc, :], dc == 0, dc == 7,
                                     [wgB, tTB], [gpB], inc=(dc == 7))
                            for dc in range(8):
                                kk.mm(up_[:, :], wu[:, dc, fc * 128:(fc + 1) * 128], tTt[:, dc, :], dc == 0, dc == 7,
                                     [wuB, tTB], [upB], inc=(dc == 7))
                            sg, sgB = sgR.next()
                            kk.act(sg[:], gp_[:, :], AF.Silu, [gpB], [sgB])
                            kk.tt("dve", sg[:], sg[:], cb[:], ALU.mult, [sgB, cbB], [sgB])
                            kk.tt("dve", hid[:, e8 * 2 + fc, :], up_[:, :], sg[:], ALU.mult, [upB, sgB], [hidB])
                    for j in range(4):
                        for half in range(2):
                            dn, dnB = dnR.next()
                            for k3 in range(16):
                                wd, wdB = wds[k3 // 2]
                                kk.mm(dn[:, :], hid[:, k3, j * 128:(j + 1) * 128],
                                      wd[:, k3 % 2, half * 512:(half + 1) * 512], k3 == 0, k3 == 15,
                                      [hidB, wdB], [dnB], inc=(k3 == 15))
                            kk.tt("dve", acc[:, j, half * 512:(half + 1) * 512], dn[:, :],
                                 acc[:, j, half * 512:(half + 1) * 512], ALU.add, [dnB, accB], [accB])
                return ops

            def final3(qt):
                ops = []
                kk = Defer(k, S, ops)
                tTt, tTB, cT, cTB, acc, accB = st3[qt]
                for j in range(4):
                    n = qt * 4 + j
                    ss, ssB = nt["ss"].next()
                    yo, yoB = yoR.next()
                    hn, hnB = nt["hn"].next()
                    kk.act(hn[:], acc[:, j, :], AF.Square, [accB], [hnB, ssB], accum_out=ss[:, 0:1])
                    kk.act(ss[:, 1:2], ss[:, 0:1], AF.Ln, [ssB, constB], [ssB], scale=1.0 / D, bias=cst[:, 0:1])
                    kk.act(ss[:, 2:3], ss[:, 1:2], AF.Exp, [ssB], [ssB], scale=-0.5)
                    kk.stt(yo[:], acc[:, j, :], ss[:, 2:3], gfin[:], ALU.mult, ALU.mult, [accB, ssB, gfinB], [yoB])
                    kk.dma("sp", y[n * 128:(n + 1) * 128, :], yo[:], reads=[yoB], writes=[yB[n % 4]])
                return ops

            for f in front3(0):
                f()
            for qt in range(NT):
                side = []
                if qt >= 1:
                    side += final3(qt - 1)
                if qt + 1 < NT:
                    side += front3(qt + 1)
                run_merged(experts3(qt, 0) + experts3(qt, 1), side)
            for f in final3(NT - 1):
                f()
            S.barrier()
            S.emit()
    return nc


def make_in_maps(inputs):
    f = lambda a: np.ascontiguousarray(np.asarray(a, dtype=np.float32))
    L = 0
    x = f(inputs["x"])
    mem = f(inputs["mem"])
    rep = lambda v: np.broadcast_to(f(v).reshape(1, D), (128, D))
    gains = np.ascontiguousarray(np.stack([rep(inputs["norm_mix"][L]), rep(inputs["norm_xattn"][L]),
                                           rep(inputs["norm_moe"][L]), rep(inputs["norm_final"]),
                                           rep(inputs["norm_mem"][L])], 0))
    pc = lambda v: f(v).reshape(-1, 128).T
    pp = np.zeros((128, 40), np.float32)
    cw = f(inputs["conv_w"][L])
    for c in range(4):
        for t in range(4):
            pp[:, c * 4 + t] = cw[t, c * 128:(c + 1) * 128]
    pp[:, 16:20] = pc(inputs["conv_b"][L])
    pp[:, 20:24] = pc(inputs["lru_b_a"][L].reshape(-1))
    pp[:, 24:28] = pc(inputs["lru_b_x"][L].reshape(-1))
    pp[:, 28:32] = pc(inputs["lru_lambda"][L])
    pp[:, 32:36] = pc(inputs["norm_rnn_out"][L])
    pp[:, 36:40] = pc(inputs["norm_sb_out"][L])
    wa = f(inputs["lru_w_a"][L])
    wx = f(inputs["lru_w_x"][L])
    wbd = np.zeros((128, 8, 128), np.float32)
    for c in range(4):
        for hp in range(2):
            wbd[hp * 64:(hp + 1) * 64, c, hp * 64:(hp + 1) * 64] = wa[2 * c + hp]
            wbd[hp * 64:(hp + 1) * 64, 4 + c, hp * 64:(hp + 1) * 64] = wx[2 * c + hp]
    wr = np.zeros((D, 20), np.float32)
    wr[:, 0:4] = f(inputs["w_group_router"][L])
    wer = f(inputs["w_expert_router"][L])
    for g in range(4):
        wr[:, 4 + 4 * g:8 + 4 * g] = wer[g]
    rbv = np.concatenate([f(inputs["b_group_router"][L]).reshape(-1), f(inputs["b_expert_router"][L]).reshape(-1)])
    rb = np.ascontiguousarray(np.broadcast_to(rbv.reshape(1, 20), (128, 20)))
    shared = {
        "w_in": f(inputs["w_in"][L]), "w_out": f(inputs["w_out"][L]),
        "xa_w_q": f(inputs["xa_w_q"][L]), "xa_w_k": f(inputs["xa_w_k"][L]),
        "xa_w_v": f(inputs["xa_w_v"][L]), "xa_w_o": f(inputs["xa_w_o"][L]),
        "w_gate": f(inputs["w_gate"][L]).reshape(16, D, 256), "w_up": f(inputs["w_up"][L]).reshape(16, D, 256),
        "w_down": f(inputs["w_down"][L]).reshape(16, 256, D),
        "gains": gains, "pp": pp, "wbd": wbd, "wr": wr, "rb": rb,
    }
    return [dict(shared, x=x[b], mem=mem[b]) for b in range(x.shape[0])]


_NC_CACHE = {}


def kernel(**inputs):
    in_maps = make_in_maps(inputs)
    if "nc" not in _NC_CACHE:
        _NC_CACHE["nc"] = build()
    nc = _NC_CACHE["nc"]
    res = run_bass_kernel_spmd(nc, in_maps, core_ids=list(range(8)))
    out = np.stack([np.asarray(r["y"], dtype=np.float32) for r in res.results], 0)
    return out
```
